# Optimizing a Trainium2 kernel written in Bass

```python
import math
import jax, jax.numpy as jnp
from jax import lax
import numpy as np

D_MODEL = 4096
BATCH = 4
SEQ = 4096
DEPTH = 1

HY_WIDTH = D_MODEL // 2
HY_ORDER = 2
HY_SHORT = 3
HY_POS_EMB = 33
HY_FILTER_HIDDEN = 64
HY_DECAY_TARGET = 1e-2
HY_FAST_DECAY = 0.3
HY_SLOW_DECAY = 1.5
HY_MOD_SHIFT = 0.05
HY_FILTER_OUT_SCALE = 0.1
ML_HEADS = 8
ML_DV = (D_MODEL // 2) // ML_HEADS
ML_DK = ML_DV // 2
ML_WIDTH = ML_HEADS * ML_DV
ML_CHUNK = 64
COL_Q = (HY_ORDER + 1) * HY_WIDTH
COL_K = COL_Q + ML_HEADS * ML_DK
COL_V = COL_K + ML_HEADS * ML_DK
COL_O = COL_V + ML_WIDTH
COL_IF = COL_O + ML_WIDTH
COL_GATE = COL_IF + 4 * ML_HEADS
N_COLS = COL_GATE + 2 * D_MODEL
MOE_GROUPS = 8
MOE_PER_GROUP = 8
MOE_EXPERTS = MOE_GROUPS * MOE_PER_GROUP
MOE_TOPK = 2
MOE_HIDDEN = (D_MODEL * 3) // 16
MOE_BLOCK = 128
DEEPNORM_ALPHA = (2.0 * DEPTH) ** 0.25
DEEPNORM_BETA = (8.0 * DEPTH) ** -0.25
LN_EPS = 1e-5

kernel_name = 'hybrid_hyena_mlstm_hmoe_deepnorm'


def layer_norm(x, g, b):
    xf = x.astype(jnp.float32)
    mu = jnp.mean(xf, axis=-1, keepdims=True)
    var = jnp.mean(jnp.square(xf - mu), axis=-1, keepdims=True)
    return ((xf - mu) * lax.rsqrt(var + LN_EPS) * g + b).astype(x.dtype)


def short_conv_centred(u, w, b):
    pad = HY_SHORT // 2
    L = u.shape[1]
    up = jnp.pad(u, ((0, 0), (pad, pad), (0, 0)))
    out = b
    for j in range(HY_SHORT):
        out = out + up[:, j:j + L] * w[j]
    return out


def hyena_filters(L, f_w1, f_b1, f_fr1, f_w2, f_b2, f_fr2, f_w3, f_b3, f_fr3, f_wout):
    f32 = jnp.float32
    t = jnp.linspace(0.0, 1.0, L, dtype=f32)[:, None]
    bands = (HY_POS_EMB - 1) // 2
    freqs = jnp.linspace(1e-4, bands - 1, bands, dtype=f32)[None, :]
    w = (2.0 * math.pi / L) * jnp.arange(L, dtype=f32)[:, None]
    feats = jnp.concatenate([t, jnp.cos(freqs * w), -jnp.sin(freqs * w)], axis=-1)
    h = jnp.sin(f_fr1 * (feats @ f_w1 + f_b1))
    h = jnp.sin(f_fr2 * (h @ f_w2 + f_b2))
    h = jnp.sin(f_fr3 * (h @ f_w3 + f_b3))
    h = (h @ f_wout).astype(f32).reshape(L, 2, HY_ORDER, HY_WIDTH)
    deltas = jnp.abs(jnp.linspace(math.log(HY_DECAY_TARGET) / HY_SLOW_DECAY,
                                  math.log(HY_DECAY_TARGET) / HY_FAST_DECAY,
                                  HY_WIDTH, dtype=f32))
    window = jnp.exp(-t * deltas[None, :]) + HY_MOD_SHIFT
    return h * window[:, None, None, :]


def bidir_long_conv(u, h_fwd, h_bwd):
    L, C = h_fwd.shape
    k = jnp.concatenate([h_fwd, jnp.zeros((1, C), h_fwd.dtype), h_bwd[:0:-1]], axis=0)
    U = jnp.fft.rfft(u.astype(jnp.float32), n=2 * L, axis=1)
    K = jnp.fft.rfft(k, n=2 * L, axis=0)
    y = jnp.fft.irfft(U * K[None], n=2 * L, axis=1)[:, :L]
    return y.astype(u.dtype)


def hyena_branch(z_hy, conv_w, conv_b, filt, bias):
    u = short_conv_centred(z_hy, conv_w, conv_b)
    x1, x2, v = jnp.split(u, 3, axis=-1)
    for o, gate in enumerate((x1, x2)):
        v = gate * (bidir_long_conv(v, filt[:, 0, o], filt[:, 1, o]) + v * bias[o])
    return v


def mlstm_direction(q, k, v, log_i, log_f):
    B, H, L, _ = q.shape
    T = ML_CHUNK
    nC = L // T

    def chunks(a):
        return jnp.moveaxis(a.reshape(a.shape[:2] + (nC, T) + a.shape[3:]), 2, 0)

    xs = (chunks(q), chunks(k), chunks(v), chunks(log_i), chunks(log_f))
    within = jnp.tril(jnp.ones((T, T), dtype=bool))

    def step(carry, inp):
        C, n, m = carry
        qc, kc, vc, li, lf = inp
        b = jnp.cumsum(lf, axis=-1)
        d_log = jnp.where(within, b[..., :, None] - b[..., None, :] + li[..., None, :], -jnp.inf)
        inter = b + m[..., None]
        m_t = jnp.maximum(inter, jnp.max(d_log, axis=-1))
        scores = jnp.einsum('bhtd,bhsd->bhts', qc, kc) * jnp.exp(d_log - m_t[..., None])
        w_inter = jnp.exp(inter - m_t)
        num = (jnp.einsum('bhts,bhsv->bhtv', scores, vc)
               + w_inter[..., None] * jnp.einsum('bhtd,bhdv->bhtv', qc, C))
        den = jnp.sum(scores, axis=-1) + w_inter * jnp.einsum('bhtd,bhd->bht', qc, n)
        h = num / jnp.maximum(jnp.abs(den), jnp.exp(-m_t))[..., None]
        b_last = b[..., -1]
        w_log = b_last[..., None] - b + li
        m_new = jnp.maximum(b_last + m, jnp.max(w_log, axis=-1))
        kw = kc * jnp.exp(w_log - m_new[..., None])[..., None]
        decay = jnp.exp(b_last + m - m_new)
        C_new = decay[..., None, None] * C + jnp.einsum('bhsd,bhsv->bhdv', kw, vc)
        n_new = decay[..., None] * n + jnp.sum(kw, axis=2)
        return (C_new, n_new, m_new), h

    f32 = jnp.float32
    init = (jnp.zeros((B, H, ML_DK, ML_DV), f32), jnp.zeros((B, H, ML_DK), f32), jnp.zeros((B, H), f32))
    _, hs = lax.scan(step, init, xs)
    return jnp.moveaxis(hs, 0, 2).reshape(B, H, L, ML_DV)


def token_mixers(x, w_in, hy_conv_w, hy_conv_b, hy_f_w1, hy_f_b1, hy_f_fr1, hy_f_w2, hy_f_b2,
                 hy_f_fr2, hy_f_w3, hy_f_b3, hy_f_fr3, hy_f_wout, hy_bias, ml_gate_bias,
                 ml_norm_g, p_hy, p_ml, w_out):
    B, L, _ = x.shape
    f32 = jnp.float32
    z = x @ w_in
    filt = hyena_filters(L, hy_f_w1, hy_f_b1, hy_f_fr1, hy_f_w2, hy_f_b2, hy_f_fr2,
                         hy_f_w3, hy_f_b3, hy_f_fr3, hy_f_wout)
    y_hy = hyena_branch(z[..., :COL_Q], hy_conv_w, hy_conv_b, filt, hy_bias).astype(x.dtype)
    def heads(a, d):
        return a.reshape(B, L, ML_HEADS, d).transpose(0, 2, 1, 3).astype(f32)
    q = heads(z[..., COL_Q:COL_K], ML_DK) * (ML_DK ** -0.5)
    k = heads(z[..., COL_K:COL_V], ML_DK)
    v = heads(z[..., COL_V:COL_O], ML_DV)
    o = z[..., COL_O:COL_IF].astype(f32)
    g = (z[..., COL_IF:COL_GATE].astype(f32).reshape(B, L, 4, ML_HEADS) + ml_gate_bias).transpose(2, 0, 3, 1)
    rev = lambda a: jnp.flip(a, axis=2)
    h_fwd = mlstm_direction(q, k, v, g[0], jax.nn.log_sigmoid(g[1]))
    h_bwd = rev(mlstm_direction(rev(q), rev(k), rev(v), rev(g[2]), rev(jax.nn.log_sigmoid(g[3]))))
    h = h_fwd + h_bwd
    mu = jnp.mean(h, axis=-1, keepdims=True)
    var = jnp.mean(jnp.square(h - mu), axis=-1, keepdims=True)
    hn = ((h - mu) * lax.rsqrt(var + LN_EPS)).transpose(0, 2, 1, 3).reshape(B, L, ML_WIDTH) * ml_norm_g
    y_ml = (jax.nn.sigmoid(o) * hn).astype(x.dtype)
    gate_hy = jax.nn.sigmoid(z[..., COL_GATE:COL_GATE + D_MODEL])
    gate_ml = jax.nn.sigmoid(z[..., COL_GATE + D_MODEL:])
    merged = gate_hy * (y_hy @ p_hy) + gate_ml * (y_ml @ p_ml)
    return merged @ w_out


def hierarchical_moe(x, router_w1, router_b1, router_w2, router_b2, exp_w1, exp_w3, exp_w2):
    B, L, D = x.shape
    N = B * L
    f32 = jnp.float32
    xt = x.reshape(N, D)
    lg1 = (xt @ router_w1).astype(f32) + router_b1
    p1 = jax.nn.softmax(lg1, axis=-1)
    g_sel = jnp.argmax(lg1, axis=-1)
    p_group = jnp.take_along_axis(p1, g_sel[:, None], axis=1)[:, 0]
    lg2 = ((xt @ router_w2).astype(f32) + router_b2).reshape(N, MOE_GROUPS, MOE_PER_GROUP)
    lg2 = jnp.take_along_axis(lg2, g_sel[:, None, None], axis=1)[:, 0]
    top_p, top_j = lax.top_k(jax.nn.softmax(lg2, axis=-1), MOE_TOPK)
    top_p = top_p / jnp.sum(top_p, axis=-1, keepdims=True)
    weight = p_group[:, None] * top_p
    eid = g_sel[:, None].astype(jnp.int32) * MOE_PER_GROUP + top_j.astype(jnp.int32)
    M = N * MOE_TOPK
    eid_f = eid.reshape(M)
    tok_f = jnp.repeat(jnp.arange(N, dtype=jnp.int32), MOE_TOPK)
    w_f = weight.reshape(M)
    order = jnp.argsort(eid_f)
    e_sorted = eid_f[order]
    counts = jnp.bincount(eid_f, length=MOE_EXPERTS)
    padded = ((counts + MOE_BLOCK - 1) // MOE_BLOCK) * MOE_BLOCK
    start = jnp.cumsum(counts) - counts
    pend = jnp.cumsum(padded)
    pstart = pend - padded
    dest = pstart[e_sorted] + (jnp.arange(M, dtype=jnp.int32) - start[e_sorted])
    n_blocks = -(-M // MOE_BLOCK) + MOE_EXPERTS
    n_slots = n_blocks * MOE_BLOCK
    slot_tok = jnp.full((n_slots,), N, jnp.int32).at[dest].set(tok_f[order])
    slot_w = jnp.zeros((n_slots,), f32).at[dest].set(w_f[order])
    blk_e = jnp.clip(jnp.searchsorted(pend, jnp.arange(n_blocks) * MOE_BLOCK, side='right'), 0, MOE_EXPERTS - 1)
    x_pad = jnp.concatenate([xt, jnp.zeros((1, D), xt.dtype)], axis=0)

    def expert_block(args):
        toks, e = args
        xb = x_pad[toks]
        hb = jax.nn.silu(xb @ exp_w1[e]) * (xb @ exp_w3[e])
        return hb @ exp_w2[e]

    yb = lax.map(expert_block, (slot_tok.reshape(n_blocks, MOE_BLOCK), blk_e))
    contrib = yb.reshape(n_slots, D) * slot_w[:, None].astype(yb.dtype)
    y = jnp.zeros((N + 1, D), x.dtype).at[slot_tok].add(contrib.astype(x.dtype))[:N]
    return y.reshape(B, L, D)


def setup_inputs(seed: int = 0) -> dict:
    key = jax.random.key(seed)
    ks = jax.random.split(key, 32)
    f32 = jnp.float32
    nrm = lambda k, shape, s: jax.random.normal(k, shape, f32) * s
    D = D_MODEL
    fh = HY_FILTER_HIDDEN
    gate_bias = jnp.stack([
        nrm(ks[16], (ML_HEADS,), 0.1),
        jnp.linspace(3.0, 6.0, ML_HEADS, dtype=f32) + nrm(ks[17], (ML_HEADS,), 0.1),
        nrm(ks[18], (ML_HEADS,), 0.1),
        jnp.linspace(3.0, 6.0, ML_HEADS, dtype=f32) + nrm(ks[19], (ML_HEADS,), 0.1),
    ])
    return {
        'x': nrm(ks[0], (BATCH, SEQ, D), 1.0),
        'w_in': nrm(ks[1], (D, N_COLS), D ** -0.5),
        'hy_conv_w': nrm(ks[2], (HY_SHORT, (HY_ORDER + 1) * HY_WIDTH), HY_SHORT ** -0.5),
        'hy_conv_b': nrm(ks[3], ((HY_ORDER + 1) * HY_WIDTH,), 0.01),
        'hy_f_w1': nrm(ks[4], (HY_POS_EMB, fh), HY_POS_EMB ** -0.5),
        'hy_f_b1': nrm(ks[5], (fh,), 0.01),
        'hy_f_fr1': 1.0 + nrm(ks[6], (fh,), 0.01),
        'hy_f_w2': nrm(ks[7], (fh, fh), fh ** -0.5),
        'hy_f_b2': nrm(ks[8], (fh,), 0.01),
        'hy_f_fr2': 1.0 + nrm(ks[9], (fh,), 0.01),
        'hy_f_w3': nrm(ks[10], (fh, fh), fh ** -0.5),
        'hy_f_b3': nrm(ks[11], (fh,), 0.01),
        'hy_f_fr3': 1.0 + nrm(ks[12], (fh,), 0.01),
        'hy_f_wout': nrm(ks[13], (fh, 2 * HY_ORDER * HY_WIDTH), HY_FILTER_OUT_SCALE * fh ** -0.5),
        'hy_bias': nrm(ks[14], (HY_ORDER, HY_WIDTH), 1.0),
        'ml_gate_bias': gate_bias,
        'ml_norm_g': 1.0 + nrm(ks[15], (ML_WIDTH,), 0.02),
        'p_hy': nrm(ks[20], (HY_WIDTH, D), DEEPNORM_BETA * HY_WIDTH ** -0.5),
        'p_ml': nrm(ks[21], (ML_WIDTH, D), DEEPNORM_BETA * ML_WIDTH ** -0.5),
        'w_out': nrm(ks[22], (D, D), DEEPNORM_BETA * D ** -0.5),
        'ln1_g': 1.0 + nrm(ks[23], (D,), 0.02),
        'ln1_b': nrm(ks[24], (D,), 0.01),
        'router_w1': nrm(ks[25], (D, MOE_GROUPS), D ** -0.5),
        'router_b1': nrm(ks[26], (MOE_GROUPS,), 0.01),
        'router_w2': nrm(ks[27], (D, MOE_EXPERTS), D ** -0.5),
        'router_b2': nrm(ks[28], (MOE_EXPERTS,), 0.01),
        'exp_w1': nrm(ks[29], (MOE_EXPERTS, D, MOE_HIDDEN), D ** -0.5),
        'exp_w3': nrm(ks[30], (MOE_EXPERTS, D, MOE_HIDDEN), D ** -0.5),
        'exp_w2': nrm(ks[31], (MOE_EXPERTS, MOE_HIDDEN, D), DEEPNORM_BETA * MOE_HIDDEN ** -0.5),
        'ln2_g': 1.0 + nrm(jax.random.fold_in(key, 101), (D,), 0.02),
        'ln2_b': nrm(jax.random.fold_in(key, 102), (D,), 0.01),
    }


def reference(x, w_in, hy_conv_w, hy_conv_b, hy_f_w1, hy_f_b1, hy_f_fr1, hy_f_w2, hy_f_b2,
              hy_f_fr2, hy_f_w3, hy_f_b3, hy_f_fr3, hy_f_wout, hy_bias, ml_gate_bias, ml_norm_g,
              p_hy, p_ml, w_out, ln1_g, ln1_b, router_w1, router_b1, router_w2, router_b2,
              exp_w1, exp_w3, exp_w2, ln2_g, ln2_b):
    h = x
    for _ in range(DEPTH):
        mix = token_mixers(h, w_in, hy_conv_w, hy_conv_b, hy_f_w1, hy_f_b1, hy_f_fr1, hy_f_w2,
                           hy_f_b2, hy_f_fr2, hy_f_w3, hy_f_b3, hy_f_fr3, hy_f_wout, hy_bias,
                           ml_gate_bias, ml_norm_g, p_hy, p_ml, w_out)
        h = layer_norm(DEEPNORM_ALPHA * h + mix, ln1_g, ln1_b)
        ffn = hierarchical_moe(h, router_w1, router_b1, router_w2, router_b2, exp_w1, exp_w3, exp_w2)
        h = layer_norm(DEEPNORM_ALPHA * h + ffn, ln2_g, ln2_b)
    return h
```

```python
import math
from contextlib import ExitStack
import numpy as np
import ml_dtypes
import concourse.bass as bass
import concourse.mybir as mybir
from concourse.bass_utils import run_bass_kernel_spmd

F32 = mybir.dt.float32
BF16 = mybir.dt.bfloat16
I32 = mybir.dt.int32
AF = mybir.ActivationFunctionType
ALU = mybir.AluOpType
AX = mybir.AxisListType

D = 4096
NTOK = 16384
L = 4096
NB = 4
NCORE = 8
NFFT = 8192
PI = math.pi


class Sched:
    NDSEM = 48

    def __init__(self, nc):
        self.nc = nc
        self.E = dict(pe=nc.tensor, act=nc.scalar, dve=nc.vector, pool=nc.gpsimd, sp=nc.sync)
        self.sem = {e: nc.alloc_semaphore("sem_" + e) for e in ["pe", "act", "dve", "pool"]}
        self.cnt = {e: 0 for e in self.sem}
        self.dsems = [nc.alloc_semaphore("dsem%d" % i) for i in range(self.NDSEM)]
        self.dcnt = [0] * self.NDSEM
        self.qpool = {"sp": list(range(0, 24)), "act": list(range(24, 40)), "pool": list(range(40, 48))}
        self.qnext = {"sp": 0, "act": 0, "pool": 0}
        self.waited = {e: {} for e in self.E}
        self.lastw = {}
        self.readers = {}
        self.pend = {e: [] for e in self.E}
        self.nins = 0

    def _semobj(self, s):
        if isinstance(s, int):
            return self.dsems[s]
        return self.sem[s]

    def _wait(self, e, tok):
        if tok is None:
            return
        s, v = tok
        if e == "pe" and s == "pe":
            return
        if self.waited[e].get(s, 0) >= v:
            return
        self.waited[e][s] = v
        self.E[e].wait_ge(self._semobj(s), v)

    def _deps(self, e, reads, writes):
        for k in reads:
            self._wait(e, self.lastw.get(k))
        for k in writes:
            self._wait(e, self.lastw.get(k))
            for t in self.readers.get(k, ()):
                self._wait(e, t)

    def _commit(self, tok, reads, writes):
        for k in reads:
            self.readers.setdefault(k, []).append(tok)
        for k in writes:
            self.lastw[k] = tok
            self.readers[k] = []

    def op(self, e, fn, reads=(), writes=(), inc=True):
        self._deps(e, reads, writes)
        ins = fn(self.E[e])
        self.nins += 1
        if not inc:
            self.pend[e].append((tuple(reads), tuple(writes)))
            return None
        self.cnt[e] += 1
        tok = (e, self.cnt[e])
        ins.then_inc(self.sem[e], 1)
        for (r, w) in self.pend[e]:
            self._commit(tok, r, w)
        self.pend[e] = []
        self._commit(tok, reads, writes)
        return tok

    def dma(self, q, out, in_, reads=(), writes=(), **kw):
        pool_ = self.qpool[q]
        i = pool_[self.qnext[q] % len(pool_)]
        self.qnext[q] += 1
        if self.dcnt[i] > 0:
            self._wait(q, (i, self.dcnt[i]))
        self._deps(q, reads, writes)
        ins = self.E[q].dma_start(out=out, in_=in_, **kw)
        self.nins += 1
        self.dcnt[i] += 16
        tok = (i, self.dcnt[i])
        ins.then_inc(self.dsems[i], 16)
        self._commit(tok, reads, writes)
        return tok

    def barrier(self):
        for e in self.E:
            for sname in self.sem:
                if self.cnt[sname] > 0:
                    self._wait(e, (sname, self.cnt[sname]))
            for i in range(self.NDSEM):
                if self.dcnt[i] > 0:
                    self._wait(e, (i, self.dcnt[i]))

    def finish(self, keys, e="sp"):
        for k in keys:
            self._wait(e, self.lastw.get(k))


_CONST = {}


def _dft_consts():
    if "dft" in _CONST:
        return _CONST["dft"]
    s = np.arange(L, dtype=np.float64)[:, None]
    k = np.arange(L, dtype=np.float64)[None, :] + 0.5
    ang = (2.0 * np.pi / NFFT) * (s * k)
    out = {}
    for nm, fn in (("c", np.cos), ("s", np.sin)):
        M = fn(ang).astype(np.float32)
        fwd = M.reshape(32, 128, 32, 128).transpose(2, 1, 0, 3)
        inv = M.reshape(32, 128, 32, 128).transpose(0, 3, 2, 1)
        out[nm + "fwd"] = np.ascontiguousarray(fwd).astype(ml_dtypes.bfloat16)
        out[nm + "inv"] = np.ascontiguousarray(inv).astype(ml_dtypes.bfloat16)
    _CONST["dft"] = out
    return out


def _feats_T():
    f32 = np.float32
    t = np.linspace(0.0, 1.0, L, dtype=f32)[:, None]
    bands = 16
    freqs = np.linspace(1e-4, bands - 1, bands, dtype=f32)[None, :]
    w = (f32(2.0 * math.pi / L) * np.arange(L, dtype=f32))[:, None]
    feats = np.concatenate([t, np.cos(freqs * w), -np.sin(freqs * w)], axis=-1).astype(f32)
    return np.ascontiguousarray(feats.T)


def _deltas():
    f32 = np.float32
    return np.abs(np.linspace(math.log(1e-2) / 1.5, math.log(1e-2) / 0.3, 2048, dtype=f32)).astype(f32)


NW1 = 1540


def build_p1(stop_after=None, nblk=None, feat=0xff, debug=False, skip_hyena=False):
    nc = bass.Bass("TRN2", target_bir_lowering=False)
    S = Sched(nc)

    def din(name, shape, dt=F32):
        return nc.dram_tensor(name, list(shape), dt, kind="ExternalInput").ap()

    xT = din("xT", [D, NTOK])
    wc = din("wc", [D, NW1])
    cw = din("cw", [128, 6, 4])
    hyb = din("hyb", [128, 2, 2])
    featsT = din("featsT", [33, L])
    fw1 = din("fw1", [33, 64])
    fw2 = din("fw2", [64, 64])
    fw3 = din("fw3", [64, 64])
    fvec = din("fvec", [64, 6])
    wout = din("wout", [64, 1024])
    delB = din("delB", [128, 256])
    tnorm = din("tnorm", [128, 32])
    cfwd = din("cfwd", [32, 128, 32, 128], BF16)
    sfwd = din("sfwd", [32, 128, 32, 128], BF16)
    cinv = din("cinv", [32, 128, 32, 128], BF16)
    sinv = din("sinv", [32, 128, 32, 128], BF16)
    identb_d = din("identb", [128, 128], BF16)
    identf_d = din("identf", [128, 128])
    gbias = din("gbias", [8, 2])
    mlg = din("mlg", [128, 256])
    maskf_d = din("maskf", [128, 128])
    maskb_d = din("maskb", [128, 128])

    yhyT = nc.dram_tensor("yhyT", [256, NTOK], F32, kind="ExternalOutput").ap()
    yml = nc.dram_tensor("yml", [NTOK, 256], F32, kind="ExternalOutput").ap()

    zc = nc.dram_tensor("zc", [6, 128, NTOK], F32).ap()
    qk = nc.dram_tensor("qk", [2, 128, NTOK], BF16).ap()
    gT = nc.dram_tensor("gT", [4, NTOK], F32).ap()
    vt = nc.dram_tensor("vt", [NTOK, 256], BF16).ap()
    ot = nc.dram_tensor("ot", [NTOK, 256], F32).ap()
    z1 = nc.dram_tensor("z1", [2, 128, NTOK], F32).ap()
    rows = nc.dram_tensor("rows", [2, 8, L], F32).ap()

    ps = [nc.alloc_psum_tensor("ps%d" % i, [128, 512], F32) for i in range(8)]
    psk = ["ps%d" % i for i in range(8)]

    identb = nc.alloc_sbuf_tensor("identb_s", [128, 128], BF16)
    identf = nc.alloc_sbuf_tensor("identf_s", [128, 128], F32)
    S.dma("sp", identb[:], identb_d, writes=["identb"])
    S.dma("sp", identf[:], identf_d, writes=["identf"])

    TB = 256
    NBLK = NTOK // TB if nblk is None else nblk
    with ExitStack() as es:
        Wb = es.enter_context(nc.sbuf_tensor("Wb", [128, 32, NW1], BF16))
        wst = [es.enter_context(nc.sbuf_tensor("wst%d" % i, [128, NW1], F32)) for i in range(2)]
        xst = [es.enter_context(nc.sbuf_tensor("xst%d" % i, [128, 8, TB], F32)) for i in range(4)]
        xb = [es.enter_context(nc.sbuf_tensor("xb%d" % i, [128, 32, TB], BF16)) for i in range(2)]
        ohy = [es.enter_context(nc.sbuf_tensor("ohy%d" % i, [128, 6, TB], F32)) for i in range(2)]
        oqk = [es.enter_context(nc.sbuf_tensor("oqk%d" % i, [128, 2, TB], BF16)) for i in range(2)]
        og = [es.enter_context(nc.sbuf_tensor("og%d" % i, [128, TB], F32)) for i in range(2)]
        ov = [es.enter_context(nc.sbuf_tensor("ov%d" % i, [128, 2, 256], BF16)) for i in range(2)]
        oo = [es.enter_context(nc.sbuf_tensor("oo%d" % i, [128, 2, 256], F32)) for i in range(2)]

        wcv = wc.rearrange("(kc p) n -> p kc n", p=128)
        for kc in range(32):
            st = wst[kc % 2]
            S.dma("sp", st[:], wcv[:, kc, :], writes=["wst%d" % (kc % 2)])
            eng = "act" if kc % 2 == 0 else "dve"
            if eng == "act":
                S.op("act", lambda e, st=st, kc=kc: e.copy(out=Wb[:, kc, :], in_=st[:]),
                     reads=["wst%d" % (kc % 2)], writes=["Wb"])
            else:
                S.op("dve", lambda e, st=st, kc=kc: e.tensor_copy(out=Wb[:, kc, :], in_=st[:]),
                     reads=["wst%d" % (kc % 2)], writes=["Wb"])

        if stop_after == "A0":
            S.finish(["Wb"]); return nc
        xTv = xT.rearrange("(kc p) n -> p kc n", p=128)
        pb = 0
        evq = 0
        for blk in range(NBLK):
            t0 = blk * TB
            xbi = blk % 2
            xbt = xb[xbi]
            for pc in range(4):
                sti = pc
                S.dma("sp", xst[sti][:], xTv[:, pc * 8:(pc + 1) * 8, t0:t0 + TB], writes=["xst%d" % sti])
                if pc % 2 == 0:
                    S.op("act", lambda e, sti=sti, pc=pc, xbt=xbt: e.copy(out=xbt[:, pc * 8:(pc + 1) * 8, :], in_=xst[sti][:]),
                         reads=["xst%d" % sti], writes=["xb%d" % xbi])
                else:
                    S.op("pool", lambda e, sti=sti, pc=pc, xbt=xbt: e.tensor_copy(out=xbt[:, pc * 8:(pc + 1) * 8, :], in_=xst[sti][:]),
                         reads=["xst%d" % sti], writes=["xb%d" % xbi])
            bi = blk % 2
            if stop_after == "A1":
                S.finish(["xb%d" % xbi]); return nc
            for ct in range(8):
                bank = pb % 8
                pb += 1
                for kc in range(32):
                    S.op("pe", lambda e, bank=bank, ct=ct, kc=kc, xbt=xbt: e.matmul(
                        ps[bank][:, 0:TB], lhsT=Wb[:, kc, ct * 128:(ct + 1) * 128], rhs=xbt[:, kc, :],
                        start=(kc == 0), stop=(kc == 31)),
                        reads=["Wb", "xb%d" % xbi], writes=[psk[bank]], inc=(kc == 31))
                if ct < 6:
                    dst = ohy[bi][:, ct, :]
                    key = "ohy%d" % bi
                else:
                    dst = oqk[bi][:, ct - 6, :]
                    key = "oqk%d" % bi
                sc = (128.0 ** -0.5) if ct == 6 else 1.0
                if evq % 2 == 0:
                    S.op("act", lambda e, dst=dst, bank=bank, sc=sc: e.mul(out=dst, in_=ps[bank][:, 0:TB], mul=sc),
                         reads=[psk[bank]], writes=[key])
                else:
                    S.op("dve", lambda e, dst=dst, bank=bank, sc=sc: e.tensor_scalar(out=dst, in0=ps[bank][:, 0:TB], scalar1=sc, scalar2=None, op0=ALU.mult),
                         reads=[psk[bank]], writes=[key])
                evq += 1
            S.dma("act", zc[:, :, t0:t0 + TB].rearrange("c p n -> p c n"), ohy[bi][:], reads=["ohy%d" % bi], writes=["zc"])
            S.dma("act", qk[:, :, t0:t0 + TB].rearrange("c p n -> p c n"), oqk[bi][:], reads=["oqk%d" % bi], writes=["qk"])
            if stop_after == "A2":
                S.finish(["zc", "qk"]); return nc
            bank = pb % 8
            pb += 1
            for kc in range(32):
                S.op("pe", lambda e, bank=bank, kc=kc, xbt=xbt: e.matmul(
                    ps[bank][:, 0:TB], lhsT=Wb[:, kc, 1412:1540], rhs=xbt[:, kc, :],
                    start=(kc == 0), stop=(kc == 31)),
                    reads=["Wb", "xb%d" % xbi], writes=[psk[bank]], inc=(kc == 31))
            S.op("dve", lambda e, bank=bank, bi=bi: e.tensor_copy(out=og[bi][:], in_=ps[bank][:, 0:TB]),
                 reads=[psk[bank]], writes=["og%d" % bi])
            S.dma("act", gT[:, t0:t0 + TB], og[bi][124:128, :], reads=["og%d" % bi], writes=["gT"])
            if stop_after == "A3":
                S.finish(["zc", "qk", "gT"]); return nc
            for sub in range(2):
                bank = pb % 8
                pb += 1
                for kc in range(32):
                    S.op("pe", lambda e, bank=bank, kc=kc, sub=sub, xbt=xbt: e.matmul(
                        ps[bank][:, :], lhsT=xbt[:, kc, sub * 128:(sub + 1) * 128], rhs=Wb[:, kc, 1024:1536],
                        start=(kc == 0), stop=(kc == 31)),
                        reads=["Wb", "xb%d" % xbi], writes=[psk[bank]], inc=(kc == 31))
                if sub == 0:
                    S.op("act", lambda e, bank=bank, sub=sub, bi=bi: e.copy(out=ov[bi][:, sub, :], in_=ps[bank][:, 0:256]),
                         reads=[psk[bank]], writes=["ov%d" % bi])
                    S.op("act", lambda e, bank=bank, sub=sub, bi=bi: e.copy(out=oo[bi][:, sub, :], in_=ps[bank][:, 256:512]),
                         reads=[psk[bank]], writes=["oo%d" % bi])
                else:
                    S.op("dve", lambda e, bank=bank, sub=sub, bi=bi: e.tensor_copy(out=ov[bi][:, sub, :], in_=ps[bank][:, 0:256]),
                         reads=[psk[bank]], writes=["ov%d" % bi])
                    S.op("dve", lambda e, bank=bank, sub=sub, bi=bi: e.tensor_copy(out=oo[bi][:, sub, :], in_=ps[bank][:, 256:512]),
                         reads=[psk[bank]], writes=["oo%d" % bi])
            S.dma("act", vt[t0:t0 + TB, :].rearrange("(s p) n -> p s n", p=128), ov[bi][:], reads=["ov%d" % bi], writes=["vt"])
            S.dma("act", ot[t0:t0 + TB, :].rearrange("(s p) n -> p s n", p=128), oo[bi][:], reads=["oo%d" % bi], writes=["ot"])

    if stop_after == "A":
        S.finish(["zc", "qk", "gT", "vt", "ot"])
        return nc

    S.barrier()
    with ExitStack() as es:
      if not skip_hyena:
        cws = es.enter_context(nc.sbuf_tensor("cws", [128, 6, 4], F32))
        zin = [es.enter_context(nc.sbuf_tensor("zin%d" % i, [128, L], F32)) for i in range(2)]
        uo = [es.enter_context(nc.sbuf_tensor("uo%d" % i, [128, L], F32)) for i in range(2)]
        S.dma("sp", cws[:], cw, writes=["cws"])
        it = 0
        for ct in range(6):
            for b in range(NB):
                i = it % 2
                it += 1
                sl = slice(b * L, (b + 1) * L)
                S.dma("sp", zin[i][:], zc[ct, :, sl], reads=["zc"], writes=["zin%d" % i])
                z_, u_ = zin[i], uo[i]
                S.op("dve", lambda e, z_=z_, u_=u_, ct=ct: e.tensor_scalar(out=u_[:], in0=z_[:], scalar1=cws[:, ct, 1:2], scalar2=cws[:, ct, 3:4], op0=ALU.mult, op1=ALU.add),
                     reads=["zin%d" % i, "cws"], writes=["uo%d" % i])
                S.op("dve", lambda e, z_=z_, u_=u_, ct=ct: e.scalar_tensor_tensor(out=u_[:, 1:L], in0=z_[:, 0:L - 1], scalar=cws[:, ct, 0:1], in1=u_[:, 1:L], op0=ALU.mult, op1=ALU.add),
                     reads=["zin%d" % i, "cws", "uo%d" % i], writes=["uo%d" % i])
                S.op("dve", lambda e, z_=z_, u_=u_, ct=ct: e.scalar_tensor_tensor(out=u_[:, 0:L - 1], in0=z_[:, 1:L], scalar=cws[:, ct, 2:3], in1=u_[:, 0:L - 1], op0=ALU.mult, op1=ALU.add),
                     reads=["zin%d" % i, "cws", "uo%d" % i], writes=["uo%d" % i])
                S.dma("sp", zc[ct, :, sl], uo[i][:], reads=["uo%d" % i], writes=["zc"])

    if stop_after == "B":
        S.finish(["zc"])
        return nc

    S.barrier()
    with ExitStack() as es:
      if not skip_hyena:
        h3 = es.enter_context(nc.sbuf_tensor("h3", [64, L], BF16))
        with ExitStack() as es2:
            fT = es2.enter_context(nc.sbuf_tensor("fT", [33, L], F32))
            w1s = es2.enter_context(nc.sbuf_tensor("w1s", [33, 64], F32))
            w2s = es2.enter_context(nc.sbuf_tensor("w2s", [64, 64], F32))
            w3s = es2.enter_context(nc.sbuf_tensor("w3s", [64, 64], F32))
            fv = es2.enter_context(nc.sbuf_tensor("fv", [64, 6], F32))
            frb = es2.enter_context(nc.sbuf_tensor("frb", [64, 3], F32))
            ha = es2.enter_context(nc.sbuf_tensor("ha", [64, L], F32))
            hb2 = es2.enter_context(nc.sbuf_tensor("hb2", [64, L], F32))
            arg = es2.enter_context(nc.sbuf_tensor("arg", [64, 512], F32))
            tm1 = es2.enter_context(nc.sbuf_tensor("tm1", [64, 512], F32))
            tm2 = es2.enter_context(nc.sbuf_tensor("tm2", [64, 512], F32))
            S.dma("sp", fT[:], featsT, writes=["fT"])
            S.dma("sp", w1s[:], fw1, writes=["w1s"])
            S.dma("sp", w2s[:], fw2, writes=["w2s"])
            S.dma("sp", w3s[:], fw3, writes=["w3s"])
            S.dma("sp", fv[:], fvec, writes=["fv"])
            for l in range(3):
                S.op("dve", lambda e, l=l: e.tensor_tensor(out=frb[:, l:l + 1], in0=fv[:, 2 * l:2 * l + 1], in1=fv[:, 2 * l + 1:2 * l + 2], op=ALU.mult),
                     reads=["fv"], writes=["frb"])
            layers = [(w1s, "w1s", fT, "fT", 33, ha, "ha"), (w2s, "w2s", ha, "ha", 64, hb2, "hb2"), (w3s, "w3s", hb2, "hb2", 64, ha, "ha")]
            for l, (wt, wk, src, sk, K, dst, dk) in enumerate(layers):
                for cb in range(8):
                    cs = slice(cb * 512, (cb + 1) * 512)
                    bank = cb % 2
                    S.op("pe", lambda e, wt=wt, src=src, K=K, cs=cs, bank=bank: e.matmul(ps[bank][0:64, :], lhsT=wt[0:K, :], rhs=src[0:K, cs], start=True, stop=True),
                         reads=[wk, sk], writes=[psk[bank]])
                    S.op("act", lambda e, bank=bank, l=l: e.activation(out=arg[:], in_=ps[bank][0:64, :], func=AF.Identity, bias=frb[:, l:l + 1], scale=fv[:, 2 * l + 1:2 * l + 2]),
                         reads=[psk[bank], "frb", "fv"], writes=["arg"])
                    S.op("dve", lambda e: e.tensor_scalar(out=tm1[:], in0=arg[:], scalar1=PI, scalar2=-2.0 * PI, op0=ALU.is_gt, op1=ALU.mult),
                         reads=["arg"], writes=["tm1"])
                    S.op("dve", lambda e: e.tensor_scalar(out=tm2[:], in0=arg[:], scalar1=-PI, scalar2=2.0 * PI, op0=ALU.is_lt, op1=ALU.mult),
                         reads=["arg"], writes=["tm2"])
                    S.op("dve", lambda e: e.tensor_tensor(out=tm1[:], in0=tm1[:], in1=tm2[:], op=ALU.add),
                         reads=["tm1", "tm2"], writes=["tm1"])
                    S.op("dve", lambda e: e.tensor_tensor(out=arg[:], in0=arg[:], in1=tm1[:], op=ALU.add),
                         reads=["arg", "tm1"], writes=["arg"])
                    S.op("act", lambda e, dst=dst, cs=cs: e.activation(out=dst[:, cs], in_=arg[:], func=AF.Sin),
                         reads=["arg"], writes=[dk])
            S.op("dve", lambda e: e.tensor_copy(out=h3[:], in_=ha[:]), reads=["ha"], writes=["h3"])

        big1 = es.enter_context(nc.sbuf_tensor("big1", [128, 32, 512], BF16))
        big2 = es.enter_context(nc.sbuf_tensor("big2", [128, 32, 512], BF16))
        DT = es.enter_context(nc.sbuf_tensor("DT", [128, 32, 512], BF16))
        Kre = es.enter_context(nc.sbuf_tensor("Kre", [128, 32, 256], BF16))
        Kim = es.enter_context(nc.sbuf_tensor("Kim", [128, 32, 256], BF16))
        cblk = [es.enter_context(nc.sbuf_tensor("cblk%d" % i, [128, 32, 128], BF16)) for i in range(2)]
        sblk = [es.enter_context(nc.sbuf_tensor("sblk%d" % i, [128, 32, 128], BF16)) for i in range(2)]
        hyb_s = es.enter_context(nc.sbuf_tensor("hyb_s", [128, 2, 2], F32))
        S.dma("sp", hyb_s[:], hyb, writes=["hyb_s"])
        wosf = es.enter_context(nc.sbuf_tensor("wosf", [64, 1024], F32))
        wos = es.enter_context(nc.sbuf_tensor("wos", [64, 1024], BF16))
        delS = es.enter_context(nc.sbuf_tensor("delS", [128, 256], F32))
        tnS = es.enter_context(nc.sbuf_tensor("tnS", [128, 32], F32))
        tnN = es.enter_context(nc.sbuf_tensor("tnN", [128, 32], F32))
        win = es.enter_context(nc.sbuf_tensor("win", [128, 256], F32))
        hfw = es.enter_context(nc.sbuf_tensor("hfw", [128, 256], F32))
        hbw = es.enter_context(nc.sbuf_tensor("hbw", [128, 256], F32))
        S.dma("sp", wosf[:], wout, writes=["wosf"])
        S.op("dve", lambda e: e.tensor_copy(out=wos[:], in_=wosf[:]), reads=["wosf"], writes=["wos"])
        S.dma("sp", delS[:], delB, writes=["delS"])
        S.dma("sp", tnS[:], tnorm, writes=["tnS"])
        S.op("dve", lambda e: e.tensor_scalar(out=tnN[:], in0=tnS[:], scalar1=-1.0, scalar2=None, op0=ALU.mult), reads=["tnS"], writes=["tnN"])

        scr = es.enter_context(nc.sbuf_tensor("scr", [128, 4096], F32))
        RK = ["scr%d" % i for i in range(8)]
        ytm = [scr[:, 0:512], scr[:, 512:1024]]
        ytk = [RK[0], RK[1]]
        def g3(i):
            return scr[:, i * 512:(i + 1) * 512].rearrange("p (q n) -> p q n", q=4)
        gx = [g3(2), g3(3)]; gxk = [RK[2], RK[3]]
        gv = [g3(4), g3(5)]; gvk = [RK[4], RK[5]]
        gz = [g3(6), g3(7)]; gzk = [RK[6], RK[7]]
        t1 = es.enter_context(nc.sbuf_tensor("t1", [128, 512], F32))
        t2 = es.enter_context(nc.sbuf_tensor("t2", [128, 512], F32))
        t3 = es.enter_context(nc.sbuf_tensor("t3", [128, 512], F32))
        t4 = es.enter_context(nc.sbuf_tensor("t4", [128, 512], F32))

        Fsum = big1[:, :, 0:256]
        Fdif = big1[:, :, 256:512]

        cnt = {"blk": 0}

        def load_blocks(ca, sa, idx):
            i = cnt["blk"] % 2
            cnt["blk"] += 1
            S.dma("sp", cblk[i][:], ca[idx], writes=["cblk%d" % i])
            S.dma("sp", sblk[i][:], sa[idx], writes=["sblk%d" % i])
            return i

        for o in range(2):
            for tt in range(32):
                bank = tt % 2
                S.op("pe", lambda e, bank=bank, tt=tt, o=o: e.matmul(ps[bank][:, :], lhsT=h3[:, tt * 128:(tt + 1) * 128], rhs=wos[:, o * 512:(o + 1) * 512], start=True, stop=True),
                     reads=["h3", "wos"], writes=[psk[bank]])
                S.op("act", lambda e, tt=tt: e.activation(out=win[:], in_=delS[:], func=AF.Exp, scale=tnN[:, tt:tt + 1]),
                     reads=["delS", "tnN"], writes=["win"])
                S.op("dve", lambda e, bank=bank: e.scalar_tensor_tensor(out=hfw[:], in0=win[:], scalar=0.05, in1=ps[bank][:, 0:256], op0=ALU.add, op1=ALU.mult),
                     reads=["win", psk[bank]], writes=["hfw"])
                S.op("dve", lambda e, bank=bank: e.scalar_tensor_tensor(out=hbw[:], in0=win[:], scalar=0.05, in1=ps[bank][:, 256:512], op0=ALU.add, op1=ALU.mult),
                     reads=["win", psk[bank]], writes=["hbw"])
                if tt == 0:
                    S.op("dve", lambda e: e.memset(hbw[0:1, :], 0.0), reads=[], writes=["hbw"])
                S.op("dve", lambda e, tt=tt: e.tensor_tensor(out=Fsum[:, tt, :], in0=hfw[:], in1=hbw[:], op=ALU.add),
                     reads=["hfw", "hbw"], writes=["big1"])
                S.op("pool", lambda e, tt=tt: e.tensor_tensor(out=Fdif[:, tt, :], in0=hbw[:], in1=hfw[:], op=ALU.subtract),
                     reads=["hfw", "hbw"], writes=["big1"])
            nxt = load_blocks(cfwd, sfwd, 0)
            for kt in range(32):
                cur = nxt
                if kt + 1 < 32:
                    nxt = load_blocks(cfwd, sfwd, kt + 1)
                for sc in range(32):
                    S.op("pe", lambda e, cur=cur, sc=sc: e.matmul(ps[2][:, 0:256], lhsT=cblk[cur][:, sc, :], rhs=Fsum[:, sc, :], start=(sc == 0), stop=(sc == 31)),
                         reads=["cblk%d" % cur, "big1"], writes=[psk[2]], inc=(sc == 31))
                for sc in range(32):
                    S.op("pe", lambda e, cur=cur, sc=sc: e.matmul(ps[3][:, 0:256], lhsT=sblk[cur][:, sc, :], rhs=Fdif[:, sc, :], start=(sc == 0), stop=(sc == 31)),
                         reads=["sblk%d" % cur, "big1"], writes=[psk[3]], inc=(sc == 31))
                S.op("act", lambda e, kt=kt: e.copy(out=Kre[:, kt, :], in_=ps[2][:, 0:256]), reads=[psk[2]], writes=["Kre"])
                S.op("dve", lambda e, kt=kt: e.tensor_copy(out=Kim[:, kt, :], in_=ps[3][:, 0:256]), reads=[psk[3]], writes=["Kim"])

            src = zc[4:6] if o == 0 else z1
            gate = zc[0:2] if o == 0 else zc[2:4]
            for pg in range(2):
                for q4 in range(4):
                    for bb in range(2):
                        for ct in range(2):
                            r = bb * 2 + ct
                            tb = (pg * 2 + bb) * L + q4 * 1024
                            S.dma("sp", scr[:, r * 1024:(r + 1) * 1024], src[ct, :, tb:tb + 1024], reads=["zc", "z1"], writes=[RK[2 * r], RK[2 * r + 1]])
                    for j in range(8):
                        sc = q4 * 8 + j
                        bank = 4 + (sc % 2)
                        for r in range(4):
                            S.op("pe", lambda e, r=r, j=j, bank=bank: e.transpose(out=ps[bank][:, r * 128:(r + 1) * 128], in_=scr[:, r * 1024 + j * 128:r * 1024 + (j + 1) * 128], identity=identf[:]),
                                 reads=[RK[2 * r + (j // 4)], "identf"], writes=[psk[bank]], inc=(r == 3))
                        if sc % 2 == 0:
                            S.op("dve", lambda e, sc=sc, bank=bank: e.tensor_copy(out=DT[:, sc, :], in_=ps[bank][:, :]), reads=[psk[bank]], writes=["DT"])
                        else:
                            S.op("act", lambda e, sc=sc, bank=bank: e.copy(out=DT[:, sc, :], in_=ps[bank][:, :]), reads=[psk[bank]], writes=["DT"])
                nxt = load_blocks(cfwd, sfwd, 0)
                for kt in range(32):
                    cur = nxt
                    if kt + 1 < 32:
                        nxt = load_blocks(cfwd, sfwd, kt + 1)
                    for sc in range(32):
                        S.op("pe", lambda e, cur=cur, sc=sc: e.matmul(ps[0][:, :], lhsT=cblk[cur][:, sc, :], rhs=DT[:, sc, :], start=(sc == 0), stop=(sc == 31)),
                             reads=["cblk%d" % cur, "DT"], writes=[psk[0]], inc=(sc == 31))
                    for sc in range(32):
                        S.op("pe", lambda e, cur=cur, sc=sc: e.matmul(ps[1][:, :], lhsT=sblk[cur][:, sc, :], rhs=DT[:, sc, :], start=(sc == 0), stop=(sc == 31)),
                             reads=["sblk%d" % cur, "DT"], writes=[psk[1]], inc=(sc == 31))
                    for bb in range(2):
                        cs = slice(bb * 256, (bb + 1) * 256)
                        S.op("dve", lambda e, kt=kt, cs=cs: e.tensor_tensor(out=t1[:, cs], in0=ps[0][:, cs], in1=Kre[:, kt, :], op=ALU.mult), reads=[psk[0], "Kre"], writes=["t1"])
                        S.op("dve", lambda e, kt=kt, cs=cs: e.tensor_tensor(out=t2[:, cs], in0=ps[1][:, cs], in1=Kim[:, kt, :], op=ALU.mult), reads=[psk[1], "Kim"], writes=["t2"])
                        S.op("dve", lambda e, kt=kt, cs=cs: e.tensor_tensor(out=t3[:, cs], in0=ps[1][:, cs], in1=Kre[:, kt, :], op=ALU.mult), reads=[psk[1], "Kre"], writes=["t3"])
                        S.op("dve", lambda e, kt=kt, cs=cs: e.tensor_tensor(out=t4[:, cs], in0=ps[0][:, cs], in1=Kim[:, kt, :], op=ALU.mult), reads=[psk[0], "Kim"], writes=["t4"])
                    S.op("pool", lambda e, kt=kt: e.tensor_tensor(out=big1[:, kt, :], in0=t1[:], in1=t2[:], op=ALU.add), reads=["t1", "t2"], writes=["big1"])
                    S.op("pool", lambda e, kt=kt: e.tensor_tensor(out=big2[:, kt, :], in0=t3[:], in1=t4[:], op=ALU.subtract), reads=["t3", "t4"], writes=["big2"])
                nxt = load_blocks(cinv, sinv, 0)
                for nt in range(32):
                    cur = nxt
                    if nt + 1 < 32:
                        nxt = load_blocks(cinv, sinv, nt + 1)
                    bank = 2 + (nt % 2)
                    for kc in range(32):
                        S.op("pe", lambda e, cur=cur, kc=kc, bank=bank: e.matmul(ps[bank][:, :], lhsT=cblk[cur][:, kc, :], rhs=big1[:, kc, :], start=(kc == 0), stop=False),
                             reads=["cblk%d" % cur, "big1"], writes=[psk[bank]], inc=False)
                    for kc in range(32):
                        S.op("pe", lambda e, cur=cur, kc=kc, bank=bank: e.matmul(ps[bank][:, :], lhsT=sblk[cur][:, kc, :], rhs=big2[:, kc, :], start=False, stop=(kc == 31)),
                             reads=["sblk%d" % cur, "big2"], writes=[psk[bank]], inc=(kc == 31))
                    yi = nt % 2
                    S.op("act", lambda e, bank=bank, yi=yi: e.mul(out=ytm[yi], in_=ps[bank][:, :], mul=2.0 / NFFT), reads=[psk[bank]], writes=[ytk[yi]])
                    for bb in range(2):
                        tb = (pg * 2 + bb) * L + nt * 128
                        S.dma("sp", gx[yi][:, bb * 2:bb * 2 + 2, :], gate[:, :, tb:tb + 128].rearrange("c p n -> p c n"), reads=["zc"], writes=[gxk[yi]])
                        S.dma("sp", gv[yi][:, bb * 2:bb * 2 + 2, :], src[:, :, tb:tb + 128].rearrange("c p n -> p c n"), reads=["zc", "z1"], writes=[gvk[yi]])
                    tbank = 6 + (nt % 2)
                    for q in range(4):
                        S.op("pe", lambda e, q=q, yi=yi, tbank=tbank: e.transpose(out=ps[tbank][:, q * 128:(q + 1) * 128], in_=ytm[yi][:, q * 128:(q + 1) * 128], identity=identf[:]),
                             reads=[ytk[yi], "identf"], writes=[psk[tbank]], inc=(q == 3))
                    for q in range(4):
                        ct = q % 2
                        S.op("dve", lambda e, q=q, yi=yi, tbank=tbank, ct=ct, o=o: e.scalar_tensor_tensor(out=gz[yi][:, q, :], in0=gv[yi][:, q, :], scalar=hyb_s[:, ct, o:o + 1], in1=ps[tbank][:, q * 128:(q + 1) * 128], op0=ALU.mult, op1=ALU.add),
                             reads=[gvk[yi], "hyb_s", psk[tbank]], writes=[gzk[yi]])
                    S.op("pool", lambda e, yi=yi: e.tensor_tensor(out=gz[yi], in0=gz[yi], in1=gx[yi], op=ALU.mult), reads=[gzk[yi], gxk[yi]], writes=[gzk[yi]])
                    dstT = z1 if o == 0 else yhyT.rearrange("(c p) n -> c p n", p=128)
                    dk = "z1" if o == 0 else "yhyT"
                    for bb in range(2):
                        tb = (pg * 2 + bb) * L + nt * 128
                        S.dma("act", dstT[:, :, tb:tb + 128].rearrange("c p n -> p c n"), gz[yi][:, bb * 2:bb * 2 + 2, :], reads=[gzk[yi]], writes=[dk])

    if stop_after == "D":
        S.finish(["yhyT"])
        return nc

    S.barrier()
    dbg = None
    if debug:
        dbg = (nc.dram_tensor("dbg_rows", [2, 8, L], F32, kind="ExternalOutput").ap(),
               nc.dram_tensor("dbg_h", [NTOK, 256], F32, kind="ExternalOutput").ap())
    build_mlstm(nc, S, ps, psk, identf, qk, gT, vt, ot, rows, gbias, mlg, maskf_d, maskb_d, yml, dbg)
    S.barrier()
    S.finish(["yhyT", "yml"])
    return nc


psb = None


def build_mlstm(nc, S, ps, psk, identf, qk, gT, vt, ot, rows, gbias, mlg, maskf_d, maskb_d, yml, dbg=None):
    with ExitStack() as es:
        GI = es.enter_context(nc.sbuf_tensor("GI", [8, L], F32))
        GF = es.enter_context(nc.sbuf_tensor("GF", [8, L], F32))
        ONE = es.enter_context(nc.sbuf_tensor("ONE", [8, L], F32))
        LF = es.enter_context(nc.sbuf_tensor("LF", [8, L], F32))
        PP = es.enter_context(nc.sbuf_tensor("PP", [8, L], F32))
        TA = es.enter_context(nc.sbuf_tensor("TA", [8, L], F32))
        gb = es.enter_context(nc.sbuf_tensor("gb", [8, 2], F32))
        S.dma("sp", gb[:], gbias, writes=["gb"])
        gTv = gT.rearrange("g (b t) -> g b t", b=NB)
        for d in range(2):
            S.dma("sp", GI[d * 4:(d + 1) * 4, :], gTv[2 * d], reads=["gT"], writes=["GI"])
            S.dma("sp", GF[d * 4:(d + 1) * 4, :], gTv[2 * d + 1], reads=["gT"], writes=["GF"])
        S.op("pool", lambda e: e.memset(ONE[:], 1.0), writes=["ONE"])
        S.op("dve", lambda e: e.tensor_scalar(out=GI[:], in0=GI[:], scalar1=gb[:, 0:1], scalar2=None, op0=ALU.add), reads=["GI", "gb"], writes=["GI"])
        S.op("dve", lambda e: e.tensor_scalar(out=GF[:], in0=GF[:], scalar1=gb[:, 1:2], scalar2=None, op0=ALU.add), reads=["GF", "gb"], writes=["GF"])
        S.op("act", lambda e: e.activation(out=LF[:], in_=GF[:], func=AF.Exp, scale=-1.0), reads=["GF"], writes=["LF"])
        S.op("act", lambda e: e.activation(out=LF[:], in_=LF[:], func=AF.Ln, bias=1.0), reads=["LF"], writes=["LF"])
        S.op("dve", lambda e: e.tensor_scalar(out=LF[:], in0=LF[:], scalar1=-1.0, scalar2=None, op0=ALU.mult), reads=["LF"], writes=["LF"])
        S.op("dve", lambda e: e.tensor_tensor_scan(out=PP[:], data0=ONE[:], data1=LF[:], initial=0.0, op0=ALU.mult, op1=ALU.add), reads=["ONE", "LF"], writes=["PP"])
        S.op("dve", lambda e: e.tensor_tensor(out=TA[:], in0=GI[:], in1=PP[:], op=ALU.subtract), reads=["GI", "PP"], writes=["TA"])
        S.dma("sp", rows[0, 0:4, :], PP[0:4, :], reads=["PP"], writes=["rows"])
        S.dma("sp", rows[1, 0:4, :], TA[0:4, :], reads=["TA"], writes=["rows"])
        S.op("dve", lambda e: e.tensor_tensor(out=LF[:], in0=LF[:], in1=PP[:], op=ALU.subtract), reads=["LF", "PP"], writes=["LF"])
        S.op("dve", lambda e: e.tensor_tensor(out=GF[:], in0=GI[:], in1=LF[:], op=ALU.subtract), reads=["GI", "LF"], writes=["GF"])
        S.dma("sp", rows[0, 4:8, :], LF[4:8, :], reads=["LF"], writes=["rows"])
        S.dma("sp", rows[1, 4:8, :], GF[4:8, :], reads=["GF"], writes=["rows"])
        if dbg is not None:
            S.dma("sp", dbg[0][0, 0:4, :], PP[0:4, :], reads=["PP"], writes=["dbg_rows"])
            S.dma("sp", dbg[0][1, 0:4, :], TA[0:4, :], reads=["TA"], writes=["dbg_rows"])
            S.dma("sp", dbg[0][0, 4:8, :], LF[4:8, :], reads=["LF"], writes=["dbg_rows"])
            S.dma("sp", dbg[0][1, 4:8, :], GF[4:8, :], reads=["GF"], writes=["dbg_rows"])
        S.barrier()

    with ExitStack() as es:
        R = [es.enter_context(nc.sbuf_tensor("R%d" % i, [33, L], F32)) for i in range(2)]
        Lt = [es.enter_context(nc.sbuf_tensor("Lt%d" % i, [33, L], F32)) for i in range(2)]
        kT = es.enter_context(nc.sbuf_tensor("kT", [128, L], BF16))
        qT = es.enter_context(nc.sbuf_tensor("qT", [128, L], BF16))
        va = es.enter_context(nc.sbuf_tensor("va", [128, 32, 258], BF16))
        mk = [es.enter_context(nc.sbuf_tensor("mk%d" % i, [128, 128], F32)) for i in range(2)]
        mlgs = es.enter_context(nc.sbuf_tensor("mlgs", [128, 256], F32))
        Dt = [es.enter_context(nc.sbuf_tensor("Dt%d" % i, [128, 128], F32)) for i in range(2)]
        PT = [es.enter_context(nc.sbuf_tensor("PT%d" % i, [128, 128], BF16)) for i in range(2)]
        hacc = es.enter_context(nc.sbuf_tensor("hacc", [128, 256], F32))
        den = es.enter_context(nc.sbuf_tensor("den", [128, 2], F32))
        den2 = es.enter_context(nc.sbuf_tensor("den2", [128, 2], F32))
        osb = [es.enter_context(nc.sbuf_tensor("osb%d" % i, [128, 256], F32)) for i in range(2)]
        yo = [es.enter_context(nc.sbuf_tensor("yo%d" % i, [128, 256], F32)) for i in range(2)]
        st6 = es.enter_context(nc.sbuf_tensor("st6", [128, 6], F32))
        mv = es.enter_context(nc.sbuf_tensor("mv", [128, 4], F32))
        S.dma("sp", mk[0][:], maskf_d, writes=["mk0"])
        S.dma("sp", mk[1][:], maskb_d, writes=["mk1"])
        S.dma("sp", mlgs[:], mlg, writes=["mlgs"])
        for d in range(2):
            S.op("pool", lambda e, d=d: e.memset(R[d][:], 0.0), writes=["R%d" % d])
            S.op("pool", lambda e, d=d: e.memset(Lt[d][:], 0.0), writes=["L%d" % d])
            S.op("pool", lambda e, d=d: e.memset(R[d][32:33, :], 1.0), writes=["R%d" % d])
            S.op("pool", lambda e, d=d: e.memset(Lt[d][0:1, :], 1.0), writes=["L%d" % d])
        S.op("pool", lambda e: e.memset(va[:, :, 256:258], 1.0), writes=["va"])
        pi = 0
        for b in range(NB):
            tb0 = b * L
            for d in range(2):
                S.dma("sp", R[d][0:1, :], rows[0, d * 4 + b:d * 4 + b + 1, :], reads=["rows"], writes=["R%d" % d])
                S.dma("sp", Lt[d][32:33, :], rows[1, d * 4 + b:d * 4 + b + 1, :], reads=["rows"], writes=["L%d" % d])
            S.dma("sp", qT[:], qk[0, :, tb0:tb0 + L], reads=["qk"], writes=["qT"])
            S.dma("sp", kT[:], qk[1, :, tb0:tb0 + L], reads=["qk"], writes=["kT"])
            S.dma("sp", va[:, :, 0:256], vt[tb0:tb0 + L, :].rearrange("(j p) n -> p j n", p=128), reads=["vt"], writes=["va"])
            for tt in range(32):
                ts = slice(tt * 128, (tt + 1) * 128)
                oi = tt % 2
                S.dma("sp", osb[oi][:], ot[tb0 + tt * 128:tb0 + (tt + 1) * 128, :], reads=["ot"], writes=["osb%d" % oi])
                for d in range(2):
                    Js = list(range(0, tt + 1)) if d == 0 else list(range(tt, 32))
                    accb = 6 + d
                    for ji, J in enumerate(Js):
                        js = slice(J * 128, (J + 1) * 128)
                        p = pi % 2
                        pi += 1
                        ab, sb_ = 0 + p, 2 + p
                        S.op("pe", lambda e, d=d, js=js, ts=ts, ab=ab: e.matmul(ps[ab][:, 0:128], lhsT=Lt[d][0:33, js], rhs=R[d][0:33, ts], start=True, stop=True),
                             reads=["L%d" % d, "R%d" % d], writes=[psk[ab]])
                        S.op("pe", lambda e, js=js, ts=ts, sb_=sb_: e.matmul(ps[sb_][:, 0:128], lhsT=kT[:, js], rhs=qT[:, ts], start=True, stop=True),
                             reads=["kT", "qT"], writes=[psk[sb_]])
                        if J == tt:
                            S.op("dve", lambda e, p=p, ab=ab, d=d: e.tensor_tensor(out=Dt[p][:], in0=ps[ab][:, 0:128], in1=mk[d][:], op=ALU.add),
                                 reads=[psk[ab], "mk%d" % d], writes=["Dt%d" % p])
                            S.op("act", lambda e, p=p: e.activation(out=Dt[p][:], in_=Dt[p][:], func=AF.Exp), reads=["Dt%d" % p], writes=["Dt%d" % p])
                        else:
                            S.op("act", lambda e, p=p, ab=ab: e.activation(out=Dt[p][:], in_=ps[ab][:, 0:128], func=AF.Exp), reads=[psk[ab]], writes=["Dt%d" % p])
                        S.op("dve", lambda e, p=p, sb_=sb_: e.tensor_tensor(out=PT[p][:], in0=ps[sb_][:, 0:128], in1=Dt[p][:], op=ALU.mult),
                             reads=[psk[sb_], "Dt%d" % p], writes=["PT%d" % p])
                        S.op("pe", lambda e, p=p, J=J, accb=accb, ji=ji, n=len(Js): e.matmul(ps[accb][:, 0:258], lhsT=PT[p][:], rhs=va[:, J, :], start=(ji == 0), stop=(ji == n - 1)),
                             reads=["PT%d" % p, "va"], writes=[psk[accb]], inc=True)
                    S.op("dve", lambda e, accb=accb, d=d: e.tensor_scalar(out=den2[:, 0:1], in0=ps[accb][:, 256:257], scalar1=-1.0, scalar2=1.0, op0=ALU.mult, op1=ALU.max), reads=[psk[accb]], writes=["den2"])
                    S.op("dve", lambda e, accb=accb, d=d: e.tensor_scalar(out=den2[:, 1:2], in0=ps[accb][:, 256:257], scalar1=1.0, scalar2=None, op0=ALU.max), reads=[psk[accb]], writes=["den2"])
                    S.op("dve", lambda e, d=d: e.tensor_tensor(out=den[:, d:d + 1], in0=den2[:, 0:1], in1=den2[:, 1:2], op=ALU.max), reads=["den2"], writes=["den"])
                    S.op("dve", lambda e, d=d: e.reciprocal(out=den[:, d:d + 1], in_=den[:, d:d + 1]), reads=["den"], writes=["den"])
                    if d == 0:
                        S.op("dve", lambda e, accb=accb: e.tensor_scalar(out=hacc[:], in0=ps[accb][:, 0:256], scalar1=den[:, 0:1], scalar2=None, op0=ALU.mult), reads=[psk[accb], "den"], writes=["hacc"])
                    else:
                        S.op("dve", lambda e, accb=accb: e.scalar_tensor_tensor(out=hacc[:], in0=ps[accb][:, 0:256], scalar=den[:, 1:2], in1=hacc[:], op0=ALU.mult, op1=ALU.add), reads=[psk[accb], "den", "hacc"], writes=["hacc"])
                if dbg is not None:
                    S.dma("sp", dbg[1][tb0 + tt * 128:tb0 + (tt + 1) * 128, :], hacc[:], reads=["hacc"], writes=["dbg_h"])
                S.op("dve", lambda e: e.bn_stats(out=st6[:], in_=hacc[:]), reads=["hacc"], writes=["st6"])
                S.op("dve", lambda e: e.bn_aggr(out=mv[:, 0:2], in_=st6[:]), reads=["st6"], writes=["mv"])
                S.op("act", lambda e: e.activation(out=mv[:, 2:3], in_=mv[:, 1:2], func=AF.Sqrt, bias=1e-5), reads=["mv"], writes=["mv"])
                S.op("dve", lambda e: e.reciprocal(out=mv[:, 3:4], in_=mv[:, 2:3]), reads=["mv"], writes=["mv"])
                S.op("dve", lambda e: e.tensor_scalar(out=hacc[:], in0=hacc[:], scalar1=mv[:, 0:1], scalar2=mv[:, 3:4], op0=ALU.subtract, op1=ALU.mult), reads=["hacc", "mv"], writes=["hacc"])
                S.op("act", lambda e, oi=oi: e.activation(out=osb[oi][:], in_=osb[oi][:], func=AF.Sigmoid), reads=["osb%d" % oi], writes=["osb%d" % oi])
                S.op("pool", lambda e: e.tensor_tensor(out=hacc[:], in0=hacc[:], in1=mlgs[:], op=ALU.mult), reads=["hacc", "mlgs"], writes=["hacc"])
                S.op("pool", lambda e, oi=oi: e.tensor_tensor(out=yo[oi][:], in0=hacc[:], in1=osb[oi][:], op=ALU.mult), reads=["hacc", "osb%d" % oi], writes=["yo%d" % oi])
                S.dma("sp", yml[tb0 + tt * 128:tb0 + (tt + 1) * 128, :], yo[oi][:], reads=["yo%d" % oi], writes=["yml"])


COL_Q = 6144
COL_K = COL_Q + 1024
COL_V = COL_K + 1024
COL_O = COL_V + 2048
COL_IF = COL_O + 2048
COL_GATE = COL_IF + 32


def p1_in_maps(inp):
    f32 = np.float32
    x = np.asarray(inp["x"], dtype=f32).reshape(NTOK, D)
    xT = np.ascontiguousarray(x.T)
    w_in = np.asarray(inp["w_in"], dtype=f32)
    dft = _dft_consts()
    featsT = _feats_T()
    deltas = _deltas()
    tl = np.linspace(0.0, 1.0, L, dtype=f32)
    tnorm = np.ascontiguousarray(tl.reshape(32, 128).T)
    identf = np.eye(128, dtype=f32)
    identb = identf.astype(ml_dtypes.bfloat16)
    ii = np.arange(128)
    maskf = np.where(ii[:, None] <= ii[None, :], 0.0, -30000.0).astype(f32)
    maskb = np.where(ii[:, None] >= ii[None, :], 0.0, -30000.0).astype(f32)
    conv_w = np.asarray(inp["hy_conv_w"], dtype=f32)
    conv_b = np.asarray(inp["hy_conv_b"], dtype=f32)
    hy_bias = np.asarray(inp["hy_bias"], dtype=f32)
    wout_full = np.asarray(inp["hy_f_wout"], dtype=f32).reshape(64, 2, 2, 2048)
    gate_bias = np.asarray(inp["ml_gate_bias"], dtype=f32)
    ml_g = np.asarray(inp["ml_norm_g"], dtype=f32)
    fvec = np.stack([np.asarray(inp[k], dtype=f32) for k in ("hy_f_b1", "hy_f_fr1", "hy_f_b2", "hy_f_fr2", "hy_f_b3", "hy_f_fr3")], axis=1)
    maps = []
    for c in range(NCORE):
        ch = np.arange(c * 256, (c + 1) * 256)
        cols = np.concatenate([ch, 2048 + ch, 4096 + ch,
                               COL_Q + c * 128 + np.arange(128), COL_K + c * 128 + np.arange(128),
                               COL_V + c * 256 + np.arange(256), COL_O + c * 256 + np.arange(256),
                               COL_IF + np.arange(4) * 8 + c])
        wc = np.ascontiguousarray(w_in[:, cols])
        hc = np.concatenate([ch, 2048 + ch, 4096 + ch])
        cwa = np.concatenate([conv_w[:, hc], conv_b[None, hc]], axis=0)
        cw = np.ascontiguousarray(cwa.reshape(4, 6, 128).transpose(2, 1, 0))
        hyb = np.ascontiguousarray(hy_bias[:, ch].reshape(2, 2, 128).transpose(2, 1, 0))
        wo = np.ascontiguousarray(wout_full[:, :, :, ch].transpose(0, 2, 1, 3).reshape(64, 1024))
        gb = np.zeros((8, 2), f32)
        for d in range(2):
            for b in range(NB):
                gb[d * 4 + b, 0] = gate_bias[2 * d, c]
                gb[d * 4 + b, 1] = gate_bias[2 * d + 1, c]
        maps.append(dict(
            xT=xT, wc=wc, cw=cw, hyb=hyb, featsT=featsT,
            fw1=np.asarray(inp["hy_f_w1"], dtype=f32), fw2=np.asarray(inp["hy_f_w2"], dtype=f32),
            fw3=np.asarray(inp["hy_f_w3"], dtype=f32), fvec=np.ascontiguousarray(fvec), wout=wo,
            delB=np.ascontiguousarray(np.broadcast_to(deltas[ch][None, :], (128, 256))), tnorm=tnorm,
            cfwd=dft["cfwd"], sfwd=dft["sfwd"], cinv=dft["cinv"], sinv=dft["sinv"],
            identb=identb, identf=identf, gbias=gb,
            mlg=np.ascontiguousarray(np.broadcast_to(ml_g[c * 256:(c + 1) * 256][None, :], (128, 256))),
            maskf=maskf, maskb=maskb))
    return maps


TPC = NTOK // NCORE
ALPHA = 2.0 ** 0.25
LN_EPS = 1e-5


def _cast_rows(nc, S, es_parent, jobs, width):
    with ExitStack() as es:
        st = [es.enter_context(nc.sbuf_tensor("cst%d" % i, [128, width], F32)) for i in range(2)]
        cb = [es.enter_context(nc.sbuf_tensor("ccb%d" % i, [128, width], BF16)) for i in range(2)]
        engs = ["act", "dve", "pool"]
        for n, (src, dstfn, w) in enumerate(jobs):
            i = n % 2
            S.dma("sp", st[i][:, 0:w], src, writes=["cst%d" % i])
            e = engs[n % 3]
            if e == "act":
                S.op("act", lambda en, i=i, w=w: en.copy(out=cb[i][:, 0:w], in_=st[i][:, 0:w]), reads=["cst%d" % i], writes=["ccb%d" % i])
            else:
                S.op(e, lambda en, i=i, w=w: en.tensor_copy(out=cb[i][:, 0:w], in_=st[i][:, 0:w]), reads=["cst%d" % i], writes=["ccb%d" % i])
            dst, view = dstfn(cb[i])
            S.dma("act", dst, view, reads=["ccb%d" % i], writes=["castout%d" % (n % 8)])
    S.barrier()


def build_p2a():
    nc = bass.Bass("TRN2", target_bir_lowering=False)
    S = Sched(nc)

    def din(name, shape, dt=F32):
        return nc.dram_tensor(name, list(shape), dt, kind="ExternalInput").ap()

    yhT = din("yhT", [2048, TPC])
    ymT = din("ymT", [2048, TPC])
    xTs = din("xTs", [D, TPC])
    xs = din("xs", [TPC, D])
    wg = din("wg", [D, 2 * D])
    phy = din("phy", [2048, D])
    pml = din("pml", [2048, D])
    wo = din("wo", [D, D])
    ln1g = din("ln1g", [128, D])
    ln1b = din("ln1b", [128, D])
    rw = din("rw", [D, 72])
    rb = din("rb", [128, 72])
    iota8 = din("iota8", [128, 8])
    identf_d = din("identf", [128, 128])

    h1o = nc.dram_tensor("h1o", [TPC, D], F32, kind="ExternalOutput").ap()
    rinfo = nc.dram_tensor("rinfo", [TPC, 8], F32, kind="ExternalOutput").ap()

    yh_s = nc.dram_tensor("yh_s", [4, 128, 16, 512], BF16).ap()
    ym_s = nc.dram_tensor("ym_s", [4, 128, 16, 512], BF16).ap()
    xT_s = nc.dram_tensor("xT_s", [4, 128, 32, 512], BF16).ap()
    ph_s = nc.dram_tensor("ph_s", [32, 128, 16, 128], BF16).ap()
    pm_s = nc.dram_tensor("pm_s", [32, 128, 16, 128], BF16).ap()
    wg_s = nc.dram_tensor("wg_s", [64, 128, 32, 128], BF16).ap()
    wo_s = nc.dram_tensor("wo_s", [8, 128, 32, 512], BF16).ap()
    mT_s = nc.dram_tensor("mT_s", [16, 128, 32, 128], BF16).ap()
    mix_s = nc.dram_tensor("mix_s", [TPC, D], F32).ap()

    ps = [nc.alloc_psum_tensor("ps%d" % i, [128, 512], F32) for i in range(8)]
    psk = ["ps%d" % i for i in range(8)]
    identf = nc.alloc_sbuf_tensor("identf_s", [128, 128], F32)
    S.dma("sp", identf[:], identf_d, writes=["identf"])

    jobs = []
    for kc in range(16):
        jobs.append((yhT[kc * 128:(kc + 1) * 128, :], (lambda t, kc=kc: (yh_s[:, :, kc, :].rearrange("tb p c -> p tb c"), t[:, 0:2048].rearrange("p (tb c) -> p tb c", tb=4))), 2048))
        jobs.append((ymT[kc * 128:(kc + 1) * 128, :], (lambda t, kc=kc: (ym_s[:, :, kc, :].rearrange("tb p c -> p tb c"), t[:, 0:2048].rearrange("p (tb c) -> p tb c", tb=4))), 2048))
    for kc in range(32):
        jobs.append((xTs[kc * 128:(kc + 1) * 128, :], (lambda t, kc=kc: (xT_s[:, :, kc, :].rearrange("tb p c -> p tb c"), t[:, 0:2048].rearrange("p (tb c) -> p tb c", tb=4))), 2048))
    for kc in range(16):
        jobs.append((phy[kc * 128:(kc + 1) * 128, :], (lambda t, kc=kc: (ph_s[:, :, kc, :].rearrange("ct p c -> p ct c"), t[:, 0:4096].rearrange("p (ct c) -> p ct c", ct=32))), 4096))
        jobs.append((pml[kc * 128:(kc + 1) * 128, :], (lambda t, kc=kc: (pm_s[:, :, kc, :].rearrange("ct p c -> p ct c"), t[:, 0:4096].rearrange("p (ct c) -> p ct c", ct=32))), 4096))
    for kc in range(32):
        for hf in range(2):
            jobs.append((wg[kc * 128:(kc + 1) * 128, hf * 4096:(hf + 1) * 4096],
                         (lambda t, kc=kc, hf=hf: (wg_s[hf * 32:(hf + 1) * 32, :, kc, :].rearrange("ct p c -> p ct c"), t[:, 0:4096].rearrange("p (ct c) -> p ct c", ct=32))), 4096))
        jobs.append((wo[kc * 128:(kc + 1) * 128, :], (lambda t, kc=kc: (wo_s[:, :, kc, :].rearrange("dt p c -> p dt c"), t[:, 0:4096].rearrange("p (dt c) -> p dt c", dt=8))), 4096))
    _cast_rows(nc, S, None, jobs, 4096)

    with ExitStack() as es:
        yh = es.enter_context(nc.sbuf_tensor("yh", [128, 16, 512], BF16))
        ym = es.enter_context(nc.sbuf_tensor("ym", [128, 16, 512], BF16))
        xb = es.enter_context(nc.sbuf_tensor("xb", [128, 32, 512], BF16))
        wph = [es.enter_context(nc.sbuf_tensor("wph%d" % i, [128, 16, 128], BF16)) for i in range(2)]
        wpm = [es.enter_context(nc.sbuf_tensor("wpm%d" % i, [128, 16, 128], BF16)) for i in range(2)]
        wgh = [es.enter_context(nc.sbuf_tensor("wgh%d" % i, [128, 32, 128], BF16)) for i in range(2)]
        wgm = [es.enter_context(nc.sbuf_tensor("wgm%d" % i, [128, 32, 128], BF16)) for i in range(2)]
        sgh = es.enter_context(nc.sbuf_tensor("sgh", [128, 512], F32))
        sgm = es.enter_context(nc.sbuf_tensor("sgm", [128, 512], F32))
        ta = es.enter_context(nc.sbuf_tensor("ta", [128, 512], F32))
        tbv = es.enter_context(nc.sbuf_tensor("tbv", [128, 512], F32))
        mo = [es.enter_context(nc.sbuf_tensor("mo%d" % i, [128, 512], BF16)) for i in range(2)]

        def loadw(ct):
            i = ct % 2
            S.dma("sp", wph[i][:], ph_s[ct], writes=["wph%d" % i])
            S.dma("sp", wpm[i][:], pm_s[ct], writes=["wpm%d" % i])
            S.dma("sp", wgh[i][:], wg_s[ct], writes=["wgh%d" % i])
            S.dma("sp", wgm[i][:], wg_s[32 + ct], writes=["wgm%d" % i])

        for tb in range(4):
            S.dma("sp", yh[:], yh_s[tb], writes=["yh"])
            S.dma("sp", ym[:], ym_s[tb], writes=["ym"])
            S.dma("sp", xb[:], xT_s[tb], writes=["xb"])
            loadw(0)
            for ct in range(32):
                i = ct % 2
                if ct + 1 < 32:
                    loadw(ct + 1)
                b0 = (ct % 2) * 4
                for kc in range(16):
                    S.op("pe", lambda e, kc=kc, i=i, b0=b0: e.matmul(ps[b0][:, :], lhsT=wph[i][:, kc, :], rhs=yh[:, kc, :], start=(kc == 0), stop=(kc == 15)),
                         reads=["wph%d" % i, "yh"], writes=[psk[b0]], inc=(kc == 15))
                for kc in range(16):
                    S.op("pe", lambda e, kc=kc, i=i, b0=b0: e.matmul(ps[b0 + 1][:, :], lhsT=wpm[i][:, kc, :], rhs=ym[:, kc, :], start=(kc == 0), stop=(kc == 15)),
                         reads=["wpm%d" % i, "ym"], writes=[psk[b0 + 1]], inc=(kc == 15))
                for kc in range(32):
                    S.op("pe", lambda e, kc=kc, i=i, b0=b0: e.matmul(ps[b0 + 2][:, :], lhsT=wgh[i][:, kc, :], rhs=xb[:, kc, :], start=(kc == 0), stop=(kc == 31)),
                         reads=["wgh%d" % i, "xb"], writes=[psk[b0 + 2]], inc=(kc == 31))
                for kc in range(32):
                    S.op("pe", lambda e, kc=kc, i=i, b0=b0: e.matmul(ps[b0 + 3][:, :], lhsT=wgm[i][:, kc, :], rhs=xb[:, kc, :], start=(kc == 0), stop=(kc == 31)),
                         reads=["wgm%d" % i, "xb"], writes=[psk[b0 + 3]], inc=(kc == 31))
                S.op("act", lambda e, b0=b0: e.activation(out=sgh[:], in_=ps[b0 + 2][:, :], func=AF.Sigmoid), reads=[psk[b0 + 2]], writes=["sgh"])
                S.op("act", lambda e, b0=b0: e.activation(out=sgm[:], in_=ps[b0 + 3][:, :], func=AF.Sigmoid), reads=[psk[b0 + 3]], writes=["sgm"])
                S.op("dve", lambda e, b0=b0: e.tensor_tensor(out=ta[:], in0=ps[b0][:, :], in1=sgh[:], op=ALU.mult), reads=[psk[b0], "sgh"], writes=["ta"])
                S.op("dve", lambda e, b0=b0: e.tensor_tensor(out=tbv[:], in0=ps[b0 + 1][:, :], in1=sgm[:], op=ALU.mult), reads=[psk[b0 + 1], "sgm"], writes=["tbv"])
                S.op("pool", lambda e, i=i: e.tensor_tensor(out=mo[i][:], in0=ta[:], in1=tbv[:], op=ALU.add), reads=["ta", "tbv"], writes=["mo%d" % i])
                S.dma("act", mT_s[tb * 4:(tb + 1) * 4, :, ct, :].rearrange("tt p c -> p tt c"), mo[i][:].rearrange("p (tt c) -> p tt c", tt=4), reads=["mo%d" % i], writes=["mT_s"])
    S.barrier()

    with ExitStack() as es:
        wop = [es.enter_context(nc.sbuf_tensor("wop%d" % i, [128, 32, 512], BF16)) for i in range(2)]
        mt = [es.enter_context(nc.sbuf_tensor("mt%d" % i, [128, 32, 128], BF16)) for i in range(2)]
        mxo = [es.enter_context(nc.sbuf_tensor("mxo%d" % i, [128, 512], F32)) for i in range(2)]
        S.dma("sp", wop[0][:], wo_s[0], writes=["wop0"])
        n = 0
        for dt in range(8):
            wi = dt % 2
            if dt + 1 < 8:
                S.dma("sp", wop[(dt + 1) % 2][:], wo_s[dt + 1], writes=["wop%d" % ((dt + 1) % 2)])
            for tt in range(16):
                i = n % 2
                n += 1
                S.dma("sp", mt[i][:], mT_s[tt], reads=["mT_s"], writes=["mt%d" % i])
                bank = i
                for kc in range(32):
                    S.op("pe", lambda e, kc=kc, i=i, wi=wi, bank=bank: e.matmul(ps[bank][:, :], lhsT=mt[i][:, kc, :], rhs=wop[wi][:, kc, :], start=(kc == 0), stop=(kc == 31)),
                         reads=["mt%d" % i, "wop%d" % wi], writes=[psk[bank]], inc=(kc == 31))
                if i == 0:
                    S.op("act", lambda e, i=i, bank=bank: e.copy(out=mxo[i][:], in_=ps[bank][:, :]), reads=[psk[bank]], writes=["mxo%d" % i])
                else:
                    S.op("dve", lambda e, i=i, bank=bank: e.tensor_copy(out=mxo[i][:], in_=ps[bank][:, :]), reads=[psk[bank]], writes=["mxo%d" % i])
                S.dma("act", mix_s[tt * 128:(tt + 1) * 128, dt * 512:(dt + 1) * 512], mxo[i][:], reads=["mxo%d" % i], writes=["mix_s"])
    S.barrier()

    with ExitStack() as es:
        gB = es.enter_context(nc.sbuf_tensor("gB", [128, D], F32))
        bB = es.enter_context(nc.sbuf_tensor("bB", [128, D], F32))
        rws = es.enter_context(nc.sbuf_tensor("rws", [128, 32, 72], F32))
        rbs = es.enter_context(nc.sbuf_tensor("rbs", [128, 72], F32))
        io8 = es.enter_context(nc.sbuf_tensor("io8", [128, 8], F32))
        rt = [es.enter_context(nc.sbuf_tensor("rt%d" % i, [128, D], F32)) for i in range(2)]
        xt = [es.enter_context(nc.sbuf_tensor("xt%d" % i, [128, D], F32)) for i in range(2)]
        hT = es.enter_context(nc.sbuf_tensor("hT", [128, 32, 128], F32))
        st = es.enter_context(nc.sbuf_tensor("st", [128, 8, 6], F32))
        mv = es.enter_context(nc.sbuf_tensor("mv", [128, 4], F32))
        S.dma("sp", gB[:], ln1g, writes=["gB"])
        S.dma("sp", bB[:], ln1b, writes=["bB"])
        S.dma("sp", rws[:], rw.rearrange("(kc p) n -> p kc n", p=128), writes=["rws"])
        S.dma("sp", rbs[:], rb, writes=["rbs"])
        S.dma("sp", io8[:], iota8, writes=["io8"])
        R = _RouterTiles(nc, es)
        for tt in range(16):
            i = tt % 2
            S.dma("sp", rt[i][:], mix_s[tt * 128:(tt + 1) * 128, :], reads=["mix_s"], writes=["rt%d" % i])
            S.dma("sp", xt[i][:], xs[tt * 128:(tt + 1) * 128, :], writes=["xt%d" % i])
            _ln_tile(S, rt[i], "rt%d" % i, xt[i], "xt%d" % i, ALPHA, st, mv, gB, bB)
            S.dma("act", h1o[tt * 128:(tt + 1) * 128, :], rt[i][:], reads=["rt%d" % i], writes=["h1o"])
            for q in range(8):
                bank = 4 + (q % 2)
                for j in range(4):
                    kc = q * 4 + j
                    S.op("pe", lambda e, i=i, kc=kc, j=j, bank=bank: e.transpose(out=ps[bank][:, j * 128:(j + 1) * 128], in_=rt[i][:, kc * 128:(kc + 1) * 128], identity=identf[:]),
                         reads=["rt%d" % i, "identf"], writes=[psk[bank]], inc=(j == 3))
                if q % 2 == 0:
                    S.op("act", lambda e, q=q, bank=bank: e.copy(out=hT[:, q * 4:(q + 1) * 4, :], in_=ps[bank][:, :].rearrange("p (j n) -> p j n", j=4)), reads=[psk[bank]], writes=["hT"])
                else:
                    S.op("dve", lambda e, q=q, bank=bank: e.tensor_copy(out=hT[:, q * 4:(q + 1) * 4, :], in_=ps[bank][:, :].rearrange("p (j n) -> p j n", j=4)), reads=[psk[bank]], writes=["hT"])
            for kc in range(32):
                S.op("pe", lambda e, kc=kc: e.matmul(ps[6][:, 0:72], lhsT=hT[:, kc, :], rhs=rws[:, kc, :], start=(kc == 0), stop=(kc == 31)),
                     reads=["hT", "rws"], writes=[psk[6]], inc=(kc == 31))
            _router_tile(S, R, ps[6], psk[6], rbs, io8)
            S.dma("act", rinfo[tt * 128:(tt + 1) * 128, :], R.info[:], reads=["r_info"], writes=["rinfo"])
    S.barrier()
    S.finish(["h1o", "rinfo"])
    return nc


def _ln_tile(S, rt, rk, xt, xk, alpha, st, mv, gB, bB):
    if xt is not None:
        S.op("dve", lambda e: e.scalar_tensor_tensor(out=rt[:], in0=xt[:], scalar=alpha, in1=rt[:], op0=ALU.mult, op1=ALU.add), reads=[xk, rk], writes=[rk])
    for c in range(8):
        S.op("dve", lambda e, c=c: e.bn_stats(out=st[:, c, :], in_=rt[:, c * 512:(c + 1) * 512]), reads=[rk], writes=["st"])
    S.op("dve", lambda e: e.bn_aggr(out=mv[:, 0:2], in_=st[:].rearrange("p c s -> p (c s)")), reads=["st"], writes=["mv"])
    S.op("act", lambda e: e.activation(out=mv[:, 2:3], in_=mv[:, 1:2], func=AF.Sqrt, bias=LN_EPS), reads=["mv"], writes=["mv"])
    S.op("dve", lambda e: e.reciprocal(out=mv[:, 3:4], in_=mv[:, 2:3]), reads=["mv"], writes=["mv"])
    S.op("dve", lambda e: e.tensor_scalar(out=rt[:], in0=rt[:], scalar1=mv[:, 0:1], scalar2=mv[:, 3:4], op0=ALU.subtract, op1=ALU.mult), reads=[rk, "mv"], writes=[rk])
    S.op("pool", lambda e: e.tensor_tensor(out=rt[:], in0=rt[:], in1=gB[:], op=ALU.mult), reads=[rk, "gB"], writes=[rk])
    S.op("pool", lambda e: e.tensor_tensor(out=rt[:], in0=rt[:], in1=bB[:], op=ALU.add), reads=[rk, "bB"], writes=[rk])


class _RouterTiles:
    def __init__(self, nc, es):
        def t(name, shape):
            return es.enter_context(nc.sbuf_tensor(name, shape, F32))
        self.lg = t("r_lg", [128, 72])
        self.m = t("r_m", [128, 8])
        self.oh = t("r_oh", [128, 8])
        self.e1 = t("r_e1", [128, 8])
        self.t64 = t("r_t64", [128, 64])
        self.sel = t("r_sel", [128, 8])
        self.sel2 = t("r_sel2", [128, 8])
        self.oa = t("r_oa", [128, 8])
        self.ob = t("r_ob", [128, 8])
        self.tmp8 = t("r_tmp8", [128, 8])
        self.info = t("r_info", [128, 8])


def _router_tile(S, R, psl, pk, rbs, io8):
    K = "r_w"
    def dve(fn, reads=(), writes=()):
        S.op("dve", fn, reads=[K] + list(reads), writes=[K] + list(writes))
    def act(fn, reads=(), writes=()):
        S.op("act", fn, reads=[K] + list(reads), writes=[K] + list(writes))
    lg, m, oh, e1, t64, sel, sel2, oa, ob, tmp8, info = R.lg, R.m, R.oh, R.e1, R.t64, R.sel, R.sel2, R.oa, R.ob, R.tmp8, R.info
    dve(lambda e: e.tensor_tensor(out=lg[:], in0=psl[:, 0:72], in1=rbs[:], op=ALU.add), reads=[pk, "rbs"])
    dve(lambda e: e.tensor_reduce(out=m[:, 0:1], in_=lg[:, 0:8], axis=AX.X, op=ALU.max))
    dve(lambda e: e.tensor_scalar(out=oh[:], in0=lg[:, 0:8], scalar1=m[:, 0:1], scalar2=None, op0=ALU.is_equal))
    dve(lambda e: e.tensor_scalar(out=e1[:], in0=lg[:, 0:8], scalar1=m[:, 0:1], scalar2=None, op0=ALU.subtract))
    act(lambda e: e.activation(out=e1[:], in_=e1[:], func=AF.Exp))
    dve(lambda e: e.tensor_reduce(out=m[:, 1:2], in_=e1[:], axis=AX.X, op=ALU.add))
    dve(lambda e: e.reciprocal(out=m[:, 2:3], in_=m[:, 1:2]))
    dve(lambda e: e.tensor_tensor(out=tmp8[:], in0=oh[:], in1=io8[:], op=ALU.mult), reads=["io8"])
    dve(lambda e: e.tensor_reduce(out=info[:, 0:1], in_=tmp8[:], axis=AX.X, op=ALU.add), writes=["r_info"])
    dve(lambda e: e.tensor_scalar(out=sel[:], in0=lg[:, 8:16], scalar1=oh[:, 0:1], scalar2=None, op0=ALU.mult))
    for g in range(1, 8):
        dve(lambda e, g=g: e.scalar_tensor_tensor(out=sel[:], in0=lg[:, 8 + g * 8:16 + g * 8], scalar=oh[:, g:g + 1], in1=sel[:], op0=ALU.mult, op1=ALU.add))
    dve(lambda e: e.tensor_reduce(out=m[:, 3:4], in_=sel[:], axis=AX.X, op=ALU.max))
    dve(lambda e: e.tensor_scalar(out=oa[:], in0=sel[:], scalar1=m[:, 3:4], scalar2=None, op0=ALU.is_equal))
    dve(lambda e: e.scalar_tensor_tensor(out=sel2[:], in0=oa[:], scalar=-1e30, in1=sel[:], op0=ALU.mult, op1=ALU.add))
    dve(lambda e: e.tensor_reduce(out=m[:, 4:5], in_=sel2[:], axis=AX.X, op=ALU.max))
    dve(lambda e: e.tensor_scalar(out=ob[:], in0=sel2[:], scalar1=m[:, 4:5], scalar2=None, op0=ALU.is_equal))
    dve(lambda e: e.tensor_tensor(out=tmp8[:], in0=oa[:], in1=io8[:], op=ALU.mult), reads=["io8"])
    dve(lambda e: e.tensor_reduce(out=info[:, 1:2], in_=tmp8[:], axis=AX.X, op=ALU.add), writes=["r_info"])
    dve(lambda e: e.tensor_tensor(out=tmp8[:], in0=ob[:], in1=io8[:], op=ALU.mult), reads=["io8"])
    dve(lambda e: e.tensor_reduce(out=info[:, 2:3], in_=tmp8[:], axis=AX.X, op=ALU.add), writes=["r_info"])
    dve(lambda e: e.tensor_tensor(out=m[:, 6:7], in0=m[:, 4:5], in1=m[:, 3:4], op=ALU.subtract))
    act(lambda e: e.activation(out=m[:, 6:7], in_=m[:, 6:7], func=AF.Exp))
    dve(lambda e: e.tensor_scalar(out=m[:, 6:7], in0=m[:, 6:7], scalar1=1.0, scalar2=None, op0=ALU.add))
    dve(lambda e: e.reciprocal(out=m[:, 5:6], in_=m[:, 6:7]))
    dve(lambda e: e.tensor_scalar(out=m[:, 7:8], in0=m[:, 5:6], scalar1=-1.0, scalar2=1.0, op0=ALU.mult, op1=ALU.add))
    dve(lambda e: e.tensor_tensor(out=info[:, 3:4], in0=m[:, 5:6], in1=m[:, 2:3], op=ALU.mult), writes=["r_info"])
    dve(lambda e: e.tensor_tensor(out=info[:, 4:5], in0=m[:, 7:8], in1=m[:, 2:3], op=ALU.mult), writes=["r_info"])
    dve(lambda e: e.memset(info[:, 5:8], 0.0), writes=["r_info"])


CAP = 2560
SBK = 256
NSB = CAP // SBK
HID = 768


def build_p2b():
    nc = bass.Bass("TRN2", target_bir_lowering=False)
    S = Sched(nc)

    def din(name, shape, dt=F32):
        return nc.dram_tensor(name, list(shape), dt, kind="ExternalInput").ap()

    hgT = din("hgT", [D, CAP])
    hg = din("hg", [CAP, D])
    wrow = din("wrow", [8, CAP])
    w1g = din("w1g", [8, D, HID])
    w3g = din("w3g", [8, D, HID])
    w2g = din("w2g", [8, HID, D])
    ln2g = din("ln2g", [128, D])
    ln2b = din("ln2b", [128, D])
    og = nc.dram_tensor("og", [CAP, D], F32, kind="ExternalOutput").ap()

    w1_s = nc.dram_tensor("w1_s", [8, 6, 128, 32, 128], BF16).ap()
    w3_s = nc.dram_tensor("w3_s", [8, 6, 128, 32, 128], BF16).ap()
    w2_s = nc.dram_tensor("w2_s", [8, 2, 128, 24, 512], BF16).ap()
    hT_s = nc.dram_tensor("hT_s", [NSB, 128, 32, SBK], BF16).ap()

    ps = [nc.alloc_psum_tensor("ps%d" % i, [128, 512], F32) for i in range(8)]
    psk = ["ps%d" % i for i in range(8)]

    jobs = []
    for kc in range(32):
        jobs.append((hgT[kc * 128:(kc + 1) * 128, :], (lambda t, kc=kc: (hT_s[:, :, kc, :].rearrange("sb p c -> p sb c"), t[:, 0:CAP].rearrange("p (sb c) -> p sb c", sb=NSB))), CAP))
    for e in range(8):
        for kc in range(32):
            jobs.append((w1g[e, kc * 128:(kc + 1) * 128, :], (lambda t, e=e, kc=kc: (w1_s[e, :, :, kc, :].rearrange("ft p c -> p ft c"), t[:, 0:HID].rearrange("p (ft c) -> p ft c", ft=6))), HID))
            jobs.append((w3g[e, kc * 128:(kc + 1) * 128, :], (lambda t, e=e, kc=kc: (w3_s[e, :, :, kc, :].rearrange("ft p c -> p ft c"), t[:, 0:HID].rearrange("p (ft c) -> p ft c", ft=6))), HID))
        for ft in range(6):
            jobs.append((w2g[e, ft * 128:(ft + 1) * 128, :], (lambda t, e=e, ft=ft: (w2_s[:, e // 4, :, (e % 4) * 6 + ft, :].rearrange("dt p c -> p dt c"), t[:, 0:D].rearrange("p (dt c) -> p dt c", dt=8))), D))
    _cast_rows(nc, S, None, jobs, 4096)

    with ExitStack() as es:
        gB = es.enter_context(nc.sbuf_tensor("gB", [128, D], F32))
        bB = es.enter_context(nc.sbuf_tensor("bB", [128, D], F32))
        hTb = es.enter_context(nc.sbuf_tensor("hTb", [128, 32, SBK], BF16))
        wrb = es.enter_context(nc.sbuf_tensor("wrb", [128, 8, SBK], F32))
        HTw = es.enter_context(nc.sbuf_tensor("HTw", [128, 48, SBK], BF16))
        w1p = [es.enter_context(nc.sbuf_tensor("w1p%d" % i, [128, 32, 128], BF16)) for i in range(2)]
        w3p = [es.enter_context(nc.sbuf_tensor("w3p%d" % i, [128, 32, 128], BF16)) for i in range(2)]
        w2p = [es.enter_context(nc.sbuf_tensor("w2p%d" % i, [128, 24, 512], BF16)) for i in range(2)]
        rt = [es.enter_context(nc.sbuf_tensor("rt%d" % i, [128, D], F32)) for i in range(2)]
        sa = es.enter_context(nc.sbuf_tensor("sa", [128, SBK], F32))
        sb2 = es.enter_context(nc.sbuf_tensor("sb2", [128, SBK], F32))
        st = es.enter_context(nc.sbuf_tensor("st", [128, 8, 6], F32))
        mv = es.enter_context(nc.sbuf_tensor("mv", [128, 4], F32))
        S.dma("sp", gB[:], ln2g, writes=["gB"])
        S.dma("sp", bB[:], ln2b, writes=["bB"])
        nw = 0
        for sb in range(NSB):
            s0 = sb * SBK
            S.dma("sp", hTb[:], hT_s[sb], writes=["hTb"])
            S.dma("sp", wrb[:], wrow[:, s0:s0 + SBK].partition_broadcast(128), writes=["wrb"])
            for st_ in range(2):
                S.dma("sp", rt[st_][:], hg[s0 + st_ * 128:s0 + (st_ + 1) * 128, :], writes=["rt%d" % st_])
            for e in range(8):
                for ft in range(6):
                    i = nw % 2
                    nw += 1
                    S.dma("sp", w1p[i][:], w1_s[e, ft], writes=["w1p%d" % i])
                    S.dma("sp", w3p[i][:], w3_s[e, ft], writes=["w3p%d" % i])
                    ba, bb = 2 * i, 2 * i + 1
                    for kc in range(32):
                        S.op("pe", lambda en, kc=kc, i=i, ba=ba: en.matmul(ps[ba][:, 0:SBK], lhsT=w1p[i][:, kc, :], rhs=hTb[:, kc, :], start=(kc == 0), stop=(kc == 31)),
                             reads=["w1p%d" % i, "hTb"], writes=[psk[ba]], inc=(kc == 31))
                    for kc in range(32):
                        S.op("pe", lambda en, kc=kc, i=i, bb=bb: en.matmul(ps[bb][:, 0:SBK], lhsT=w3p[i][:, kc, :], rhs=hTb[:, kc, :], start=(kc == 0), stop=(kc == 31)),
                             reads=["w3p%d" % i, "hTb"], writes=[psk[bb]], inc=(kc == 31))
                    S.op("act", lambda en, ba=ba: en.activation(out=sa[:], in_=ps[ba][:, 0:SBK], func=AF.Silu), reads=[psk[ba]], writes=["sa"])
                    S.op("dve", lambda en, bb=bb: en.tensor_tensor(out=sb2[:], in0=ps[bb][:, 0:SBK], in1=sa[:], op=ALU.mult), reads=[psk[bb], "sa"], writes=["sb2"])
                    S.op("pool", lambda en, e=e, ft=ft: en.tensor_tensor(out=HTw[:, e * 6 + ft, :], in0=sb2[:], in1=wrb[:, e, :], op=ALU.mult), reads=["sb2", "wrb"], writes=["HTw"])
            for dt in range(8):
                for hf in range(2):
                    S.dma("sp", w2p[hf][:], w2_s[dt, hf], writes=["w2p%d" % hf])
                for st_ in range(2):
                    bank = 4 + st_
                    for hf in range(2):
                        for j in range(24):
                            S.op("pe", lambda en, hf=hf, j=j, st_=st_, bank=bank: en.matmul(ps[bank][:, :], lhsT=HTw[:, hf * 24 + j, st_ * 128:(st_ + 1) * 128], rhs=w2p[hf][:, j, :],
                                                                                      start=(hf == 0 and j == 0), stop=(hf == 1 and j == 23)),
                                 reads=["HTw", "w2p%d" % hf], writes=[psk[bank]], inc=(hf == 1 and j == 23))
                    S.op("dve", lambda en, st_=st_, bank=bank, dt=dt: en.scalar_tensor_tensor(out=rt[st_][:, dt * 512:(dt + 1) * 512], in0=rt[st_][:, dt * 512:(dt + 1) * 512], scalar=ALPHA, in1=ps[bank][:, :], op0=ALU.mult, op1=ALU.add),
                         reads=["rt%d" % st_, psk[bank]], writes=["rt%d" % st_])
            for st_ in range(2):
                _ln_tile(S, rt[st_], "rt%d" % st_, None, None, None, st, mv, gB, bB)
                S.dma("act", og[s0 + st_ * 128:s0 + (st_ + 1) * 128, :], rt[st_][:], reads=["rt%d" % st_], writes=["og"])
    S.barrier()
    S.finish(["og"])
    return nc


def p2a_in_maps(inp, yhyT, yml):
    f32 = np.float32
    x = np.asarray(inp["x"], dtype=f32).reshape(NTOK, D)
    w_in = np.asarray(inp["w_in"], dtype=f32)
    wg = np.ascontiguousarray(w_in[:, COL_GATE:])
    phy = np.asarray(inp["p_hy"], dtype=f32)
    pml = np.asarray(inp["p_ml"], dtype=f32)
    wo = np.asarray(inp["w_out"], dtype=f32)
    ln1g = np.ascontiguousarray(np.broadcast_to(np.asarray(inp["ln1_g"], dtype=f32)[None, :], (128, D)))
    ln1b = np.ascontiguousarray(np.broadcast_to(np.asarray(inp["ln1_b"], dtype=f32)[None, :], (128, D)))
    rw = np.ascontiguousarray(np.concatenate([np.asarray(inp["router_w1"], dtype=f32), np.asarray(inp["router_w2"], dtype=f32)], axis=1))
    rbv = np.concatenate([np.asarray(inp["router_b1"], dtype=f32), np.asarray(inp["router_b2"], dtype=f32)])
    rb = np.ascontiguousarray(np.broadcast_to(rbv[None, :], (128, 72)))
    iota8 = np.ascontiguousarray(np.broadcast_to(np.arange(8, dtype=f32)[None, :], (128, 8)))
    identf = np.eye(128, dtype=f32)
    maps = []
    for c in range(NCORE):
        sl = slice(c * TPC, (c + 1) * TPC)
        xs = np.ascontiguousarray(x[sl])
        maps.append(dict(
            yhT=np.ascontiguousarray(yhyT[:, sl]), ymT=np.ascontiguousarray(yml[sl].T),
            xTs=np.ascontiguousarray(xs.T), xs=xs, wg=wg, phy=phy, pml=pml, wo=wo,
            ln1g=ln1g, ln1b=ln1b, rw=rw, rb=rb, iota8=iota8, identf=identf))
    return maps


def p2b_in_maps(inp, h1, rinfo):
    f32 = np.float32
    grp = np.rint(rinfo[:, 0]).astype(np.int64)
    ja = np.rint(rinfo[:, 1]).astype(np.int64)
    jb = np.rint(rinfo[:, 2]).astype(np.int64)
    ln2g = np.ascontiguousarray(np.broadcast_to(np.asarray(inp["ln2_g"], dtype=f32)[None, :], (128, D)))
    ln2b = np.ascontiguousarray(np.broadcast_to(np.asarray(inp["ln2_b"], dtype=f32)[None, :], (128, D)))
    maps, idxs = [], []
    for g in range(NCORE):
        idx = np.nonzero(grp == g)[0]
        n = len(idx)
        if n > CAP:
            raise RuntimeError("group %d has %d tokens > capacity %d" % (g, n, CAP))
        hgm = np.zeros((CAP, D), f32)
        hgm[:n] = h1[idx]
        wrow = np.zeros((8, CAP), f32)
        ar = np.arange(n)
        wrow[ja[idx], ar] = rinfo[idx, 3]
        wrow[jb[idx], ar] = rinfo[idx, 4]
        maps.append(dict(hgT=np.ascontiguousarray(hgm.T), hg=hgm, wrow=wrow,
                         w1g=np.asarray(inp["exp_w1"][g * 8:(g + 1) * 8], dtype=f32),
                         w3g=np.asarray(inp["exp_w3"][g * 8:(g + 1) * 8], dtype=f32),
                         w2g=np.asarray(inp["exp_w2"][g * 8:(g + 1) * 8], dtype=f32),
                         ln2g=ln2g, ln2b=ln2b))
        idxs.append(idx)
    return maps, idxs


def run_phase1(inp):
    maps = p1_in_maps(inp)
    nc = build_p1()
    res = run_bass_kernel_spmd(nc, maps, core_ids=list(range(NCORE)))
    yhyT = np.concatenate([r["yhyT"] for r in res.results], axis=0)
    yml = np.concatenate([r["yml"] for r in res.results], axis=1)
    return yhyT, yml


def run_phase2a(inp, yhyT, yml):
    maps = p2a_in_maps(inp, yhyT, yml)
    nc = build_p2a()
    res = run_bass_kernel_spmd(nc, maps, core_ids=list(range(NCORE)))
    h1 = np.concatenate([r["h1o"] for r in res.results], axis=0)
    rinfo = np.concatenate([r["rinfo"] for r in res.results], axis=0)
    return h1, rinfo


def run_phase2b(inp, h1, rinfo):
    maps, idxs = p2b_in_maps(inp, h1, rinfo)
    nc = build_p2b()
    res = run_bass_kernel_spmd(nc, maps, core_ids=list(range(NCORE)))
    out = np.zeros((NTOK, D), np.float32)
    for g in range(NCORE):
        idx = idxs[g]
        out[idx] = res.results[g]["og"][:len(idx)]
    return out


def kernel(**inputs):
    yhyT, yml = run_phase1(inputs)
    h1, rinfo = run_phase2a(inputs, yhyT, yml)
    out = run_phase2b(inputs, h1, rinfo)
    return out.reshape(NB, L, D).astype(np.float32)
```

```python
import math
from contextlib import ExitStack
import numpy as np
import ml_dtypes
import concourse.bass as bass
import concourse.mybir as mybir
from concourse.bass_utils import run_bass_kernel_spmd

F32 = mybir.dt.float32
BF16 = mybir.dt.bfloat16
I32 = mybir.dt.int32
AF = mybir.ActivationFunctionType
ALU = mybir.AluOpType
AX = mybir.AxisListType

D = 4096
NTOK = 16384
L = 4096
NB = 4
NCORE = 8
NFFT = 8192
PI = math.pi


class Sched:
    NDSEM = 48

    def __init__(self, nc):
        self.nc = nc
        self.E = dict(pe=nc.tensor, act=nc.scalar, dve=nc.vector, pool=nc.gpsimd, sp=nc.sync)
        self.sem = {e: nc.alloc_semaphore("sem_" + e) for e in ["pe", "act", "dve", "pool"]}
        self.cnt = {e: 0 for e in self.sem}
        self.dsems = [nc.alloc_semaphore("dsem%d" % i) for i in range(self.NDSEM)]
        self.dcnt = [0] * self.NDSEM
        self.qpool = {"sp": list(range(0, 24)), "act": list(range(24, 40)), "pool": list(range(40, 48))}
        self.qnext = {"sp": 0, "act": 0, "pool": 0}
        self.waited = {e: {} for e in self.E}
        self.lastw = {}
        self.readers = {}
        self.pend = {e: [] for e in self.E}
        self.nins = 0

    def _semobj(self, s):
        if isinstance(s, int):
            return self.dsems[s]
        return self.sem[s]

    def _wait(self, e, tok):
        if tok is None:
            return
        s, v = tok
        if e == "pe" and s == "pe":
            return
        if self.waited[e].get(s, 0) >= v:
            return
        self.waited[e][s] = v
        self.E[e].wait_ge(self._semobj(s), v)

    def _deps(self, e, reads, writes):
        for k in reads:
            self._wait(e, self.lastw.get(k))
        for k in writes:
            self._wait(e, self.lastw.get(k))
            for t in self.readers.get(k, ()):
                self._wait(e, t)

    def _commit(self, tok, reads, writes):
        for k in reads:
            self.readers.setdefault(k, []).append(tok)
        for k in writes:
            self.lastw[k] = tok
            self.readers[k] = []

    def op(self, e, fn, reads=(), writes=(), inc=True):
        self._deps(e, reads, writes)
        ins = fn(self.E[e])
        self.nins += 1
        if not inc:
            self.pend[e].append((tuple(reads), tuple(writes)))
            return None
        self.cnt[e] += 1
        tok = (e, self.cnt[e])
        ins.then_inc(self.sem[e], 1)
        for (r, w) in self.pend[e]:
            self._commit(tok, r, w)
        self.pend[e] = []
        self._commit(tok, reads, writes)
        return tok

    def dma(self, q, out, in_, reads=(), writes=(), **kw):
        pool_ = self.qpool[q]
        i = pool_[self.qnext[q] % len(pool_)]
        self.qnext[q] += 1
        if self.dcnt[i] > 0:
            self._wait(q, (i, self.dcnt[i]))
        self._deps(q, reads, writes)
        ins = self.E[q].dma_start(out=out, in_=in_, **kw)
        self.nins += 1
        self.dcnt[i] += 16
        tok = (i, self.dcnt[i])
        ins.then_inc(self.dsems[i], 16)
        self._commit(tok, reads, writes)
        return tok

    def barrier(self):
        for e in self.E:
            for sname in self.sem:
                if self.cnt[sname] > 0:
                    self._wait(e, (sname, self.cnt[sname]))
            for i in range(self.NDSEM):
                if self.dcnt[i] > 0:
                    self._wait(e, (i, self.dcnt[i]))

    def finish(self, keys, e="sp"):
        for k in keys:
            self._wait(e, self.lastw.get(k))


_CONST = {}


def _dft_consts():
    if "dft" in _CONST:
        return _CONST["dft"]
    s = np.arange(L, dtype=np.float64)[:, None]
    k = np.arange(L, dtype=np.float64)[None, :] + 0.5
    ang = (2.0 * np.pi / NFFT) * (s * k)
    out = {}
    for nm, fn in (("c", np.cos), ("s", np.sin)):
        M = fn(ang).astype(np.float32)
        fwd = M.reshape(32, 128, 32, 128).transpose(2, 1, 0, 3)
        inv = M.reshape(32, 128, 32, 128).transpose(0, 3, 2, 1)
        out[nm + "fwd"] = np.ascontiguousarray(fwd).astype(ml_dtypes.bfloat16)
        out[nm + "inv"] = np.ascontiguousarray(inv).astype(ml_dtypes.bfloat16)
    _CONST["dft"] = out
    return out


def _feats_T():
    f32 = np.float32
    t = np.linspace(0.0, 1.0, L, dtype=f32)[:, None]
    bands = 16
    freqs = np.linspace(1e-4, bands - 1, bands, dtype=f32)[None, :]
    w = (f32(2.0 * math.pi / L) * np.arange(L, dtype=f32))[:, None]
    feats = np.concatenate([t, np.cos(freqs * w), -np.sin(freqs * w)], axis=-1).astype(f32)
    return np.ascontiguousarray(feats.T)


def _deltas():
    f32 = np.float32
    return np.abs(np.linspace(math.log(1e-2) / 1.5, math.log(1e-2) / 0.3, 2048, dtype=f32)).astype(f32)


NW1 = 1540


def build_p1(stop_after=None, nblk=None, feat=0xff, debug=False, skip_hyena=False):
    nc = bass.Bass("TRN2", target_bir_lowering=False)
    S = Sched(nc)

    def din(name, shape, dt=F32):
        return nc.dram_tensor(name, list(shape), dt, kind="ExternalInput").ap()

    xT = din("xT", [D, NTOK])
    wc = din("wc", [D, NW1])
    cw = din("cw", [128, 6, 4])
    hyb = din("hyb", [128, 2, 2])
    featsT = din("featsT", [33, L])
    fw1 = din("fw1", [33, 64])
    fw2 = din("fw2", [64, 64])
    fw3 = din("fw3", [64, 64])
    fvec = din("fvec", [64, 6])
    wout = din("wout", [64, 1024])
    delB = din("delB", [128, 256])
    tnorm = din("tnorm", [128, 32])
    cfwd = din("cfwd", [32, 128, 32, 128], BF16)
    sfwd = din("sfwd", [32, 128, 32, 128], BF16)
    cinv = din("cinv", [32, 128, 32, 128], BF16)
    sinv = din("sinv", [32, 128, 32, 128], BF16)
    identb_d = din("identb", [128, 128], BF16)
    identf_d = din("identf", [128, 128])
    gbias = din("gbias", [8, 2])
    mlg = din("mlg", [128, 256])
    mpat_d = din("mpat", [128, 8, 512])

    yhyT = nc.dram_tensor("yhyT", [256, NTOK], F32, kind="ExternalOutput").ap()
    yml = nc.dram_tensor("yml", [NTOK, 256], F32, kind="ExternalOutput").ap()

    zc = nc.dram_tensor("zc", [6, 128, NTOK], F32).ap()
    qk = nc.dram_tensor("qk", [2, 128, NTOK], BF16).ap()
    gT = nc.dram_tensor("gT", [4, NTOK], F32).ap()
    vt = nc.dram_tensor("vt", [NTOK, 256], BF16).ap()
    ot = nc.dram_tensor("ot", [NTOK, 256], F32).ap()
    z1 = nc.dram_tensor("z1", [2, 128, NTOK], F32).ap()
    rows = nc.dram_tensor("rows", [2, 8, L], F32).ap()

    ps = [nc.alloc_psum_tensor("ps%d" % i, [128, 512], F32) for i in range(8)]
    psk = ["ps%d" % i for i in range(8)]

    identb = nc.alloc_sbuf_tensor("identb_s", [128, 128], BF16)
    identf = nc.alloc_sbuf_tensor("identf_s", [128, 128], F32)
    S.dma("sp", identb[:], identb_d, writes=["identb"])
    S.dma("sp", identf[:], identf_d, writes=["identf"])

    TB = 256
    NBLK = NTOK // TB if nblk is None else nblk
    with ExitStack() as es:
        Wb = es.enter_context(nc.sbuf_tensor("Wb", [128, 32, NW1], BF16))
        wst = [es.enter_context(nc.sbuf_tensor("wst%d" % i, [128, NW1], F32)) for i in range(2)]
        xst = [es.enter_context(nc.sbuf_tensor("xst%d" % i, [128, 8, TB], F32)) for i in range(4)]
        xb = [es.enter_context(nc.sbuf_tensor("xb%d" % i, [128, 32, TB], BF16)) for i in range(2)]
        ohy = [es.enter_context(nc.sbuf_tensor("ohy%d" % i, [128, 6, TB], F32)) for i in range(2)]
        oqk = [es.enter_context(nc.sbuf_tensor("oqk%d" % i, [128, 2, TB], BF16)) for i in range(2)]
        og = [es.enter_context(nc.sbuf_tensor("og%d" % i, [128, TB], F32)) for i in range(2)]
        ov = [es.enter_context(nc.sbuf_tensor("ov%d" % i, [128, 2, 256], BF16)) for i in range(2)]
        oo = [es.enter_context(nc.sbuf_tensor("oo%d" % i, [128, 2, 256], F32)) for i in range(2)]

        wcv = wc.rearrange("(kc p) n -> p kc n", p=128)
        for kc in range(32):
            st = wst[kc % 2]
            S.dma("sp", st[:], wcv[:, kc, :], writes=["wst%d" % (kc % 2)])
            eng = "act" if kc % 2 == 0 else "dve"
            if eng == "act":
                S.op("act", lambda e, st=st, kc=kc: e.copy(out=Wb[:, kc, :], in_=st[:]),
                     reads=["wst%d" % (kc % 2)], writes=["Wb"])
            else:
                S.op("dve", lambda e, st=st, kc=kc: e.tensor_copy(out=Wb[:, kc, :], in_=st[:]),
                     reads=["wst%d" % (kc % 2)], writes=["Wb"])

        if stop_after == "A0":
            S.finish(["Wb"]); return nc
        xTv = xT.rearrange("(kc p) n -> p kc n", p=128)
        pb = 0
        evq = 0
        for blk in range(NBLK):
            t0 = blk * TB
            xbi = blk % 2
            xbt = xb[xbi]
            for pc in range(4):
                sti = pc
                S.dma("sp", xst[sti][:], xTv[:, pc * 8:(pc + 1) * 8, t0:t0 + TB], writes=["xst%d" % sti])
                if pc % 2 == 0:
                    S.op("act", lambda e, sti=sti, pc=pc, xbt=xbt: e.copy(out=xbt[:, pc * 8:(pc + 1) * 8, :], in_=xst[sti][:]),
                         reads=["xst%d" % sti], writes=["xb%d" % xbi])
                else:
                    S.op("pool", lambda e, sti=sti, pc=pc, xbt=xbt: e.tensor_copy(out=xbt[:, pc * 8:(pc + 1) * 8, :], in_=xst[sti][:]),
                         reads=["xst%d" % sti], writes=["xb%d" % xbi])
            bi = blk % 2
            if stop_after == "A1":
                S.finish(["xb%d" % xbi]); return nc
            for ct in range(8):
                bank = pb % 8
                pb += 1
                for kc in range(32):
                    S.op("pe", lambda e, bank=bank, ct=ct, kc=kc, xbt=xbt: e.matmul(
                        ps[bank][:, 0:TB], lhsT=Wb[:, kc, ct * 128:(ct + 1) * 128], rhs=xbt[:, kc, :],
                        start=(kc == 0), stop=(kc == 31)),
                        reads=["Wb", "xb%d" % xbi], writes=[psk[bank]], inc=(kc == 31))
                if ct < 6:
                    dst = ohy[bi][:, ct, :]
                    key = "ohy%d" % bi
                else:
                    dst = oqk[bi][:, ct - 6, :]
                    key = "oqk%d" % bi
                sc = (128.0 ** -0.5) if ct == 6 else 1.0
                if evq % 2 == 0:
                    S.op("act", lambda e, dst=dst, bank=bank, sc=sc: e.mul(out=dst, in_=ps[bank][:, 0:TB], mul=sc),
                         reads=[psk[bank]], writes=[key])
                else:
                    S.op("dve", lambda e, dst=dst, bank=bank, sc=sc: e.tensor_scalar(out=dst, in0=ps[bank][:, 0:TB], scalar1=sc, scalar2=None, op0=ALU.mult),
                         reads=[psk[bank]], writes=[key])
                evq += 1
            S.dma("act", zc[:, :, t0:t0 + TB].rearrange("c p n -> p c n"), ohy[bi][:], reads=["ohy%d" % bi], writes=["zc"])
            S.dma("act", qk[:, :, t0:t0 + TB].rearrange("c p n -> p c n"), oqk[bi][:], reads=["oqk%d" % bi], writes=["qk"])
            if stop_after == "A2":
                S.finish(["zc", "qk"]); return nc
            bank = pb % 8
            pb += 1
            for kc in range(32):
                S.op("pe", lambda e, bank=bank, kc=kc, xbt=xbt: e.matmul(
                    ps[bank][:, 0:TB], lhsT=Wb[:, kc, 1412:1540], rhs=xbt[:, kc, :],
                    start=(kc == 0), stop=(kc == 31)),
                    reads=["Wb", "xb%d" % xbi], writes=[psk[bank]], inc=(kc == 31))
            S.op("dve", lambda e, bank=bank, bi=bi: e.tensor_copy(out=og[bi][:], in_=ps[bank][:, 0:TB]),
                 reads=[psk[bank]], writes=["og%d" % bi])
            S.dma("act", gT[:, t0:t0 + TB], og[bi][124:128, :], reads=["og%d" % bi], writes=["gT"])
            if stop_after == "A3":
                S.finish(["zc", "qk", "gT"]); return nc
            for sub in range(2):
                bank = pb % 8
                pb += 1
                for kc in range(32):
                    S.op("pe", lambda e, bank=bank, kc=kc, sub=sub, xbt=xbt: e.matmul(
                        ps[bank][:, :], lhsT=xbt[:, kc, sub * 128:(sub + 1) * 128], rhs=Wb[:, kc, 1024:1536],
                        start=(kc == 0), stop=(kc == 31)),
                        reads=["Wb", "xb%d" % xbi], writes=[psk[bank]], inc=(kc == 31))
                if sub == 0:
                    S.op("act", lambda e, bank=bank, sub=sub, bi=bi: e.copy(out=ov[bi][:, sub, :], in_=ps[bank][:, 0:256]),
                         reads=[psk[bank]], writes=["ov%d" % bi])
                    S.op("act", lambda e, bank=bank, sub=sub, bi=bi: e.copy(out=oo[bi][:, sub, :], in_=ps[bank][:, 256:512]),
                         reads=[psk[bank]], writes=["oo%d" % bi])
                else:
                    S.op("dve", lambda e, bank=bank, sub=sub, bi=bi: e.tensor_copy(out=ov[bi][:, sub, :], in_=ps[bank][:, 0:256]),
                         reads=[psk[bank]], writes=["ov%d" % bi])
                    S.op("dve", lambda e, bank=bank, sub=sub, bi=bi: e.tensor_copy(out=oo[bi][:, sub, :], in_=ps[bank][:, 256:512]),
                         reads=[psk[bank]], writes=["oo%d" % bi])
            S.dma("act", vt[t0:t0 + TB, :].rearrange("(s p) n -> p s n", p=128), ov[bi][:], reads=["ov%d" % bi], writes=["vt"])
            S.dma("act", ot[t0:t0 + TB, :].rearrange("(s p) n -> p s n", p=128), oo[bi][:], reads=["oo%d" % bi], writes=["ot"])

    if stop_after == "A":
        S.finish(["zc", "qk", "gT", "vt", "ot"])
        return nc

    S.barrier()
    with ExitStack() as es:
      if not skip_hyena:
        cws = es.enter_context(nc.sbuf_tensor("cws", [128, 6, 4], F32))
        zin = [es.enter_context(nc.sbuf_tensor("zin%d" % i, [128, L], F32)) for i in range(2)]
        uo = [es.enter_context(nc.sbuf_tensor("uo%d" % i, [128, L], F32)) for i in range(2)]
        S.dma("sp", cws[:], cw, writes=["cws"])
        it = 0
        for ct in range(6):
            for b in range(NB):
                i = it % 2
                it += 1
                sl = slice(b * L, (b + 1) * L)
                S.dma("sp", zin[i][:], zc[ct, :, sl], reads=["zc"], writes=["zin%d" % i])
                z_, u_ = zin[i], uo[i]
                S.op("dve", lambda e, z_=z_, u_=u_, ct=ct: e.tensor_scalar(out=u_[:], in0=z_[:], scalar1=cws[:, ct, 1:2], scalar2=cws[:, ct, 3:4], op0=ALU.mult, op1=ALU.add),
                     reads=["zin%d" % i, "cws"], writes=["uo%d" % i])
                S.op("dve", lambda e, z_=z_, u_=u_, ct=ct: e.scalar_tensor_tensor(out=u_[:, 1:L], in0=z_[:, 0:L - 1], scalar=cws[:, ct, 0:1], in1=u_[:, 1:L], op0=ALU.mult, op1=ALU.add),
                     reads=["zin%d" % i, "cws", "uo%d" % i], writes=["uo%d" % i])
                S.op("dve", lambda e, z_=z_, u_=u_, ct=ct: e.scalar_tensor_tensor(out=u_[:, 0:L - 1], in0=z_[:, 1:L], scalar=cws[:, ct, 2:3], in1=u_[:, 0:L - 1], op0=ALU.mult, op1=ALU.add),
                     reads=["zin%d" % i, "cws", "uo%d" % i], writes=["uo%d" % i])
                S.dma("sp", zc[ct, :, sl], uo[i][:], reads=["uo%d" % i], writes=["zc"])

    if stop_after == "B":
        S.finish(["zc"])
        return nc

    S.barrier()
    with ExitStack() as es:
      if not skip_hyena:
        h3 = es.enter_context(nc.sbuf_tensor("h3", [64, L], BF16))
        with ExitStack() as es2:
            fT = es2.enter_context(nc.sbuf_tensor("fT", [33, L], F32))
            w1s = es2.enter_context(nc.sbuf_tensor("w1s", [33, 64], F32))
            w2s = es2.enter_context(nc.sbuf_tensor("w2s", [64, 64], F32))
            w3s = es2.enter_context(nc.sbuf_tensor("w3s", [64, 64], F32))
            fv = es2.enter_context(nc.sbuf_tensor("fv", [64, 6], F32))
            frb = es2.enter_context(nc.sbuf_tensor("frb", [64, 3], F32))
            ha = es2.enter_context(nc.sbuf_tensor("ha", [64, L], F32))
            hb2 = es2.enter_context(nc.sbuf_tensor("hb2", [64, L], F32))
            arg = es2.enter_context(nc.sbuf_tensor("arg", [64, 512], F32))
            tm1 = es2.enter_context(nc.sbuf_tensor("tm1", [64, 512], F32))
            tm2 = es2.enter_context(nc.sbuf_tensor("tm2", [64, 512], F32))
            S.dma("sp", fT[:], featsT, writes=["fT"])
            S.dma("sp", w1s[:], fw1, writes=["w1s"])
            S.dma("sp", w2s[:], fw2, writes=["w2s"])
            S.dma("sp", w3s[:], fw3, writes=["w3s"])
            S.dma("sp", fv[:], fvec, writes=["fv"])
            for l in range(3):
                S.op("dve", lambda e, l=l: e.tensor_tensor(out=frb[:, l:l + 1], in0=fv[:, 2 * l:2 * l + 1], in1=fv[:, 2 * l + 1:2 * l + 2], op=ALU.mult),
                     reads=["fv"], writes=["frb"])
            layers = [(w1s, "w1s", fT, "fT", 33, ha, "ha"), (w2s, "w2s", ha, "ha", 64, hb2, "hb2"), (w3s, "w3s", hb2, "hb2", 64, ha, "ha")]
            for l, (wt, wk, src, sk, K, dst, dk) in enumerate(layers):
                for cb in range(8):
                    cs = slice(cb * 512, (cb + 1) * 512)
                    bank = cb % 2
                    S.op("pe", lambda e, wt=wt, src=src, K=K, cs=cs, bank=bank: e.matmul(ps[bank][0:64, :], lhsT=wt[0:K, :], rhs=src[0:K, cs], start=True, stop=True),
                         reads=[wk, sk], writes=[psk[bank]])
                    S.op("act", lambda e, bank=bank, l=l: e.activation(out=arg[:], in_=ps[bank][0:64, :], func=AF.Identity, bias=frb[:, l:l + 1], scale=fv[:, 2 * l + 1:2 * l + 2]),
                         reads=[psk[bank], "frb", "fv"], writes=["arg"])
                    S.op("dve", lambda e: e.tensor_scalar(out=tm1[:], in0=arg[:], scalar1=PI, scalar2=-2.0 * PI, op0=ALU.is_gt, op1=ALU.mult),
                         reads=["arg"], writes=["tm1"])
                    S.op("dve", lambda e: e.tensor_scalar(out=tm2[:], in0=arg[:], scalar1=-PI, scalar2=2.0 * PI, op0=ALU.is_lt, op1=ALU.mult),
                         reads=["arg"], writes=["tm2"])
                    S.op("dve", lambda e: e.tensor_tensor(out=tm1[:], in0=tm1[:], in1=tm2[:], op=ALU.add),
                         reads=["tm1", "tm2"], writes=["tm1"])
                    S.op("dve", lambda e: e.tensor_tensor(out=arg[:], in0=arg[:], in1=tm1[:], op=ALU.add),
                         reads=["arg", "tm1"], writes=["arg"])
                    S.op("act", lambda e, dst=dst, cs=cs: e.activation(out=dst[:, cs], in_=arg[:], func=AF.Sin),
                         reads=["arg"], writes=[dk])
            S.op("dve", lambda e: e.tensor_copy(out=h3[:], in_=ha[:]), reads=["ha"], writes=["h3"])

        big1 = es.enter_context(nc.sbuf_tensor("big1", [128, 32, 512], BF16))
        big2 = es.enter_context(nc.sbuf_tensor("big2", [128, 32, 512], BF16))
        DT = es.enter_context(nc.sbuf_tensor("DT", [128, 32, 512], BF16))
        Kre = es.enter_context(nc.sbuf_tensor("Kre", [128, 32, 256], BF16))
        Kim = es.enter_context(nc.sbuf_tensor("Kim", [128, 32, 256], BF16))
        cblk = [es.enter_context(nc.sbuf_tensor("cblk%d" % i, [128, 32, 128], BF16)) for i in range(2)]
        sblk = [es.enter_context(nc.sbuf_tensor("sblk%d" % i, [128, 32, 128], BF16)) for i in range(2)]
        hyb_s = es.enter_context(nc.sbuf_tensor("hyb_s", [128, 2, 2], F32))
        S.dma("sp", hyb_s[:], hyb, writes=["hyb_s"])
        wosf = es.enter_context(nc.sbuf_tensor("wosf", [64, 1024], F32))
        wos = es.enter_context(nc.sbuf_tensor("wos", [64, 1024], BF16))
        delS = es.enter_context(nc.sbuf_tensor("delS", [128, 256], F32))
        tnS = es.enter_context(nc.sbuf_tensor("tnS", [128, 32], F32))
        tnN = es.enter_context(nc.sbuf_tensor("tnN", [128, 32], F32))
        win = es.enter_context(nc.sbuf_tensor("win", [128, 256], F32))
        hfw = es.enter_context(nc.sbuf_tensor("hfw", [128, 256], F32))
        hbw = es.enter_context(nc.sbuf_tensor("hbw", [128, 256], F32))
        S.dma("sp", wosf[:], wout, writes=["wosf"])
        S.op("dve", lambda e: e.tensor_copy(out=wos[:], in_=wosf[:]), reads=["wosf"], writes=["wos"])
        S.dma("sp", delS[:], delB, writes=["delS"])
        S.dma("sp", tnS[:], tnorm, writes=["tnS"])
        S.op("dve", lambda e: e.tensor_scalar(out=tnN[:], in0=tnS[:], scalar1=-1.0, scalar2=None, op0=ALU.mult), reads=["tnS"], writes=["tnN"])

        scr = es.enter_context(nc.sbuf_tensor("scr", [128, 4096], F32))
        RK = ["scr%d" % i for i in range(8)]
        ytm = [scr[:, 0:512], scr[:, 512:1024]]
        ytk = [RK[0], RK[1]]
        def g3(i):
            return scr[:, i * 512:(i + 1) * 512].rearrange("p (q n) -> p q n", q=4)
        gx = [g3(2), g3(3)]; gxk = [RK[2], RK[3]]
        gv = [g3(4), g3(5)]; gvk = [RK[4], RK[5]]
        gz = [g3(6), g3(7)]; gzk = [RK[6], RK[7]]
        t1 = es.enter_context(nc.sbuf_tensor("t1", [128, 512], F32))
        t2 = es.enter_context(nc.sbuf_tensor("t2", [128, 512], F32))
        t3 = es.enter_context(nc.sbuf_tensor("t3", [128, 512], F32))
        t4 = es.enter_context(nc.sbuf_tensor("t4", [128, 512], F32))

        Fsum = big1[:, :, 0:256]
        Fdif = big1[:, :, 256:512]

        cnt = {"blk": 0}

        def load_blocks(ca, sa, idx):
            i = cnt["blk"] % 2
            cnt["blk"] += 1
            S.dma("sp", cblk[i][:], ca[idx], writes=["cblk%d" % i])
            S.dma("sp", sblk[i][:], sa[idx], writes=["sblk%d" % i])
            return i

        for o in range(2):
            for tt in range(32):
                bank = tt % 2
                S.op("pe", lambda e, bank=bank, tt=tt, o=o: e.matmul(ps[bank][:, :], lhsT=h3[:, tt * 128:(tt + 1) * 128], rhs=wos[:, o * 512:(o + 1) * 512], start=True, stop=True),
                     reads=["h3", "wos"], writes=[psk[bank]])
                S.op("act", lambda e, tt=tt: e.activation(out=win[:], in_=delS[:], func=AF.Exp, scale=tnN[:, tt:tt + 1]),
                     reads=["delS", "tnN"], writes=["win"])
                S.op("dve", lambda e, bank=bank: e.scalar_tensor_tensor(out=hfw[:], in0=win[:], scalar=0.05, in1=ps[bank][:, 0:256], op0=ALU.add, op1=ALU.mult),
                     reads=["win", psk[bank]], writes=["hfw"])
                S.op("dve", lambda e, bank=bank: e.scalar_tensor_tensor(out=hbw[:], in0=win[:], scalar=0.05, in1=ps[bank][:, 256:512], op0=ALU.add, op1=ALU.mult),
                     reads=["win", psk[bank]], writes=["hbw"])
                if tt == 0:
                    S.op("dve", lambda e: e.memset(hbw[0:1, :], 0.0), reads=[], writes=["hbw"])
                S.op("dve", lambda e, tt=tt: e.tensor_tensor(out=Fsum[:, tt, :], in0=hfw[:], in1=hbw[:], op=ALU.add),
                     reads=["hfw", "hbw"], writes=["big1"])
                S.op("pool", lambda e, tt=tt: e.tensor_tensor(out=Fdif[:, tt, :], in0=hbw[:], in1=hfw[:], op=ALU.subtract),
                     reads=["hfw", "hbw"], writes=["big1"])
            nxt = load_blocks(cfwd, sfwd, 0)
            for kt in range(32):
                cur = nxt
                if kt + 1 < 32:
                    nxt = load_blocks(cfwd, sfwd, kt + 1)
                for sc in range(32):
                    S.op("pe", lambda e, cur=cur, sc=sc: e.matmul(ps[2][:, 0:256], lhsT=cblk[cur][:, sc, :], rhs=Fsum[:, sc, :], start=(sc == 0), stop=(sc == 31)),
                         reads=["cblk%d" % cur, "big1"], writes=[psk[2]], inc=(sc == 31))
                for sc in range(32):
                    S.op("pe", lambda e, cur=cur, sc=sc: e.matmul(ps[3][:, 0:256], lhsT=sblk[cur][:, sc, :], rhs=Fdif[:, sc, :], start=(sc == 0), stop=(sc == 31)),
                         reads=["sblk%d" % cur, "big1"], writes=[psk[3]], inc=(sc == 31))
                S.op("act", lambda e, kt=kt: e.copy(out=Kre[:, kt, :], in_=ps[2][:, 0:256]), reads=[psk[2]], writes=["Kre"])
                S.op("dve", lambda e, kt=kt: e.tensor_copy(out=Kim[:, kt, :], in_=ps[3][:, 0:256]), reads=[psk[3]], writes=["Kim"])

            src = zc[4:6] if o == 0 else z1
            gate = zc[0:2] if o == 0 else zc[2:4]
            for pg in range(2):
                for q4 in range(4):
                    for bb in range(2):
                        for ct in range(2):
                            r = bb * 2 + ct
                            tb = (pg * 2 + bb) * L + q4 * 1024
                            S.dma("sp", scr[:, r * 1024:(r + 1) * 1024], src[ct, :, tb:tb + 1024], reads=["zc", "z1"], writes=[RK[2 * r], RK[2 * r + 1]])
                    for j in range(8):
                        sc = q4 * 8 + j
                        bank = 4 + (sc % 2)
                        for r in range(4):
                            S.op("pe", lambda e, r=r, j=j, bank=bank: e.transpose(out=ps[bank][:, r * 128:(r + 1) * 128], in_=scr[:, r * 1024 + j * 128:r * 1024 + (j + 1) * 128], identity=identf[:]),
                                 reads=[RK[2 * r + (j // 4)], "identf"], writes=[psk[bank]], inc=(r == 3))
                        if sc % 2 == 0:
                            S.op("dve", lambda e, sc=sc, bank=bank: e.tensor_copy(out=DT[:, sc, :], in_=ps[bank][:, :]), reads=[psk[bank]], writes=["DT"])
                        else:
                            S.op("act", lambda e, sc=sc, bank=bank: e.copy(out=DT[:, sc, :], in_=ps[bank][:, :]), reads=[psk[bank]], writes=["DT"])
                nxt = load_blocks(cfwd, sfwd, 0)
                for kt in range(32):
                    cur = nxt
                    if kt + 1 < 32:
                        nxt = load_blocks(cfwd, sfwd, kt + 1)
                    for sc in range(32):
                        S.op("pe", lambda e, cur=cur, sc=sc: e.matmul(ps[0][:, :], lhsT=cblk[cur][:, sc, :], rhs=DT[:, sc, :], start=(sc == 0), stop=(sc == 31)),
                             reads=["cblk%d" % cur, "DT"], writes=[psk[0]], inc=(sc == 31))
                    for sc in range(32):
                        S.op("pe", lambda e, cur=cur, sc=sc: e.matmul(ps[1][:, :], lhsT=sblk[cur][:, sc, :], rhs=DT[:, sc, :], start=(sc == 0), stop=(sc == 31)),
                             reads=["sblk%d" % cur, "DT"], writes=[psk[1]], inc=(sc == 31))
                    for bb in range(2):
                        cs = slice(bb * 256, (bb + 1) * 256)
                        S.op("dve", lambda e, kt=kt, cs=cs: e.tensor_tensor(out=t1[:, cs], in0=ps[0][:, cs], in1=Kre[:, kt, :], op=ALU.mult), reads=[psk[0], "Kre"], writes=["t1"])
                        S.op("dve", lambda e, kt=kt, cs=cs: e.tensor_tensor(out=t2[:, cs], in0=ps[1][:, cs], in1=Kim[:, kt, :], op=ALU.mult), reads=[psk[1], "Kim"], writes=["t2"])
                        S.op("dve", lambda e, kt=kt, cs=cs: e.tensor_tensor(out=t3[:, cs], in0=ps[1][:, cs], in1=Kre[:, kt, :], op=ALU.mult), reads=[psk[1], "Kre"], writes=["t3"])
                        S.op("dve", lambda e, kt=kt, cs=cs: e.tensor_tensor(out=t4[:, cs], in0=ps[0][:, cs], in1=Kim[:, kt, :], op=ALU.mult), reads=[psk[0], "Kim"], writes=["t4"])
                    S.op("pool", lambda e, kt=kt: e.tensor_tensor(out=big1[:, kt, :], in0=t1[:], in1=t2[:], op=ALU.add), reads=["t1", "t2"], writes=["big1"])
                    S.op("pool", lambda e, kt=kt: e.tensor_tensor(out=big2[:, kt, :], in0=t3[:], in1=t4[:], op=ALU.subtract), reads=["t3", "t4"], writes=["big2"])
                nxt = load_blocks(cinv, sinv, 0)
                for nt in range(32):
                    cur = nxt
                    if nt + 1 < 32:
                        nxt = load_blocks(cinv, sinv, nt + 1)
                    bank = 2 + (nt % 2)
                    for kc in range(32):
                        S.op("pe", lambda e, cur=cur, kc=kc, bank=bank: e.matmul(ps[bank][:, :], lhsT=cblk[cur][:, kc, :], rhs=big1[:, kc, :], start=(kc == 0), stop=False),
                             reads=["cblk%d" % cur, "big1"], writes=[psk[bank]], inc=False)
                    for kc in range(32):
                        S.op("pe", lambda e, cur=cur, kc=kc, bank=bank: e.matmul(ps[bank][:, :], lhsT=sblk[cur][:, kc, :], rhs=big2[:, kc, :], start=False, stop=(kc == 31)),
                             reads=["sblk%d" % cur, "big2"], writes=[psk[bank]], inc=(kc == 31))
                    yi = nt % 2
                    S.op("act", lambda e, bank=bank, yi=yi: e.mul(out=ytm[yi], in_=ps[bank][:, :], mul=2.0 / NFFT), reads=[psk[bank]], writes=[ytk[yi]])
                    for bb in range(2):
                        tb = (pg * 2 + bb) * L + nt * 128
                        S.dma("sp", gx[yi][:, bb * 2:bb * 2 + 2, :], gate[:, :, tb:tb + 128].rearrange("c p n -> p c n"), reads=["zc"], writes=[gxk[yi]])
                        S.dma("sp", gv[yi][:, bb * 2:bb * 2 + 2, :], src[:, :, tb:tb + 128].rearrange("c p n -> p c n"), reads=["zc", "z1"], writes=[gvk[yi]])
                    tbank = 6 + (nt % 2)
                    for q in range(4):
                        S.op("pe", lambda e, q=q, yi=yi, tbank=tbank: e.transpose(out=ps[tbank][:, q * 128:(q + 1) * 128], in_=ytm[yi][:, q * 128:(q + 1) * 128], identity=identf[:]),
                             reads=[ytk[yi], "identf"], writes=[psk[tbank]], inc=(q == 3))
                    for q in range(4):
                        ct = q % 2
                        S.op("dve", lambda e, q=q, yi=yi, tbank=tbank, ct=ct, o=o: e.scalar_tensor_tensor(out=gz[yi][:, q, :], in0=gv[yi][:, q, :], scalar=hyb_s[:, ct, o:o + 1], in1=ps[tbank][:, q * 128:(q + 1) * 128], op0=ALU.mult, op1=ALU.add),
                             reads=[gvk[yi], "hyb_s", psk[tbank]], writes=[gzk[yi]])
                    S.op("pool", lambda e, yi=yi: e.tensor_tensor(out=gz[yi], in0=gz[yi], in1=gx[yi], op=ALU.mult), reads=[gzk[yi], gxk[yi]], writes=[gzk[yi]])
                    dstT = z1 if o == 0 else yhyT.rearrange("(c p) n -> c p n", p=128)
                    dk = "z1" if o == 0 else "yhyT"
                    for bb in range(2):
                        tb = (pg * 2 + bb) * L + nt * 128
                        S.dma("act", dstT[:, :, tb:tb + 128].rearrange("c p n -> p c n"), gz[yi][:, bb * 2:bb * 2 + 2, :], reads=[gzk[yi]], writes=[dk])

    if stop_after == "D":
        S.finish(["yhyT"])
        return nc

    S.barrier()
    dbg = None
    if debug:
        dbg = (nc.dram_tensor("dbg_rows", [2, 8, L], F32, kind="ExternalOutput").ap(),
               nc.dram_tensor("dbg_h", [NTOK, 256], F32, kind="ExternalOutput").ap())
    build_mlstm(nc, S, ps, psk, identf, qk, gT, vt, ot, rows, gbias, mlg, mpat_d, yml, dbg)
    S.barrier()
    S.finish(["yhyT", "yml"])
    return nc


psb = None


def build_mlstm(nc, S, ps, psk, identf, qk, gT, vt, ot, rows, gbias, mlg, mpat_d, yml, dbg=None):
    with ExitStack() as es:
        GI = es.enter_context(nc.sbuf_tensor("GI", [8, L], F32))
        GF = es.enter_context(nc.sbuf_tensor("GF", [8, L], F32))
        ONE = es.enter_context(nc.sbuf_tensor("ONE", [8, L], F32))
        LF = es.enter_context(nc.sbuf_tensor("LF", [8, L], F32))
        PP = es.enter_context(nc.sbuf_tensor("PP", [8, L], F32))
        TA = es.enter_context(nc.sbuf_tensor("TA", [8, L], F32))
        gb = es.enter_context(nc.sbuf_tensor("gb", [8, 2], F32))
        S.dma("sp", gb[:], gbias, writes=["gb"])
        gTv = gT.rearrange("g (b t) -> g b t", b=NB)
        for d in range(2):
            S.dma("sp", GI[d * 4:(d + 1) * 4, :], gTv[2 * d], reads=["gT"], writes=["GI"])
            S.dma("sp", GF[d * 4:(d + 1) * 4, :], gTv[2 * d + 1], reads=["gT"], writes=["GF"])
        S.op("pool", lambda e: e.memset(ONE[:], 1.0), writes=["ONE"])
        S.op("dve", lambda e: e.tensor_scalar(out=GI[:], in0=GI[:], scalar1=gb[:, 0:1], scalar2=None, op0=ALU.add), reads=["GI", "gb"], writes=["GI"])
        S.op("dve", lambda e: e.tensor_scalar(out=GF[:], in0=GF[:], scalar1=gb[:, 1:2], scalar2=None, op0=ALU.add), reads=["GF", "gb"], writes=["GF"])
        S.op("act", lambda e: e.activation(out=LF[:], in_=GF[:], func=AF.Exp, scale=-1.0), reads=["GF"], writes=["LF"])
        S.op("act", lambda e: e.activation(out=LF[:], in_=LF[:], func=AF.Ln, bias=1.0), reads=["LF"], writes=["LF"])
        S.op("dve", lambda e: e.tensor_scalar(out=LF[:], in0=LF[:], scalar1=-1.0, scalar2=None, op0=ALU.mult), reads=["LF"], writes=["LF"])
        S.op("dve", lambda e: e.tensor_tensor_scan(out=PP[:], data0=ONE[:], data1=LF[:], initial=0.0, op0=ALU.mult, op1=ALU.add), reads=["ONE", "LF"], writes=["PP"])
        S.op("dve", lambda e: e.tensor_tensor(out=TA[:], in0=GI[:], in1=PP[:], op=ALU.subtract), reads=["GI", "PP"], writes=["TA"])
        S.dma("sp", rows[0, 0:4, :], PP[0:4, :], reads=["PP"], writes=["rows"])
        S.dma("sp", rows[1, 0:4, :], TA[0:4, :], reads=["TA"], writes=["rows"])
        S.op("dve", lambda e: e.tensor_tensor(out=LF[:], in0=LF[:], in1=PP[:], op=ALU.subtract), reads=["LF", "PP"], writes=["LF"])
        S.op("dve", lambda e: e.tensor_tensor(out=GF[:], in0=GI[:], in1=LF[:], op=ALU.subtract), reads=["GI", "LF"], writes=["GF"])
        S.dma("sp", rows[0, 4:8, :], LF[4:8, :], reads=["LF"], writes=["rows"])
        S.dma("sp", rows[1, 4:8, :], GF[4:8, :], reads=["GF"], writes=["rows"])
        if dbg is not None:
            S.dma("sp", dbg[0][0, 0:4, :], PP[0:4, :], reads=["PP"], writes=["dbg_rows"])
            S.dma("sp", dbg[0][1, 0:4, :], TA[0:4, :], reads=["TA"], writes=["dbg_rows"])
            S.dma("sp", dbg[0][0, 4:8, :], LF[4:8, :], reads=["LF"], writes=["dbg_rows"])
            S.dma("sp", dbg[0][1, 4:8, :], GF[4:8, :], reads=["GF"], writes=["dbg_rows"])
        S.barrier()

    with ExitStack() as es:
        R = [es.enter_context(nc.sbuf_tensor("R%d" % i, [33, L], F32)) for i in range(2)]
        Lt = [es.enter_context(nc.sbuf_tensor("Lt%d" % i, [33, L], F32)) for i in range(2)]
        kT = es.enter_context(nc.sbuf_tensor("kT", [128, L], BF16))
        qT = es.enter_context(nc.sbuf_tensor("qT", [128, L], BF16))
        va = es.enter_context(nc.sbuf_tensor("va", [128, 32, 258], BF16))
        mp = es.enter_context(nc.sbuf_tensor("mp", [128, 8, 512], F32))
        mlgs = es.enter_context(nc.sbuf_tensor("mlgs", [128, 256], F32))
        Dt = [es.enter_context(nc.sbuf_tensor("Dt%d" % i, [128, 512], F32)) for i in range(2)]
        PT = [es.enter_context(nc.sbuf_tensor("PT%d" % i, [128, 512], BF16)) for i in range(2)]
        hacc = es.enter_context(nc.sbuf_tensor("hacc", [128, 4, 256], F32))
        den = es.enter_context(nc.sbuf_tensor("den", [128, 4], F32))
        den2 = es.enter_context(nc.sbuf_tensor("den2", [128, 2], F32))
        osb = [es.enter_context(nc.sbuf_tensor("osb%d" % i, [128, 4, 256], F32)) for i in range(2)]
        yo = [es.enter_context(nc.sbuf_tensor("yo%d" % i, [128, 4, 256], F32)) for i in range(2)]
        st6 = es.enter_context(nc.sbuf_tensor("st6", [128, 6], F32))
        mv = es.enter_context(nc.sbuf_tensor("mv", [128, 4], F32))
        S.dma("sp", mp[:], mpat_d, writes=["mp"])
        S.dma("sp", mlgs[:], mlg, writes=["mlgs"])
        for d in range(2):
            S.op("pool", lambda e, d=d: e.memset(R[d][:], 0.0), writes=["R%d" % d])
            S.op("pool", lambda e, d=d: e.memset(Lt[d][:], 0.0), writes=["L%d" % d])
            S.op("pool", lambda e, d=d: e.memset(R[d][32:33, :], 1.0), writes=["R%d" % d])
            S.op("pool", lambda e, d=d: e.memset(Lt[d][0:1, :], 1.0), writes=["L%d" % d])
        S.op("pool", lambda e: e.memset(va[:, :, 256:258], 1.0), writes=["va"])
        pi = 0
        gi = 0
        for b in range(NB):
            tb0 = b * L
            for d in range(2):
                S.dma("sp", R[d][0:1, :], rows[0, d * 4 + b:d * 4 + b + 1, :], reads=["rows"], writes=["R%d" % d])
                S.dma("sp", Lt[d][32:33, :], rows[1, d * 4 + b:d * 4 + b + 1, :], reads=["rows"], writes=["L%d" % d])
            S.dma("sp", qT[:], qk[0, :, tb0:tb0 + L], reads=["qk"], writes=["qT"])
            S.dma("sp", kT[:], qk[1, :, tb0:tb0 + L], reads=["qk"], writes=["kT"])
            S.dma("sp", va[:, :, 0:256], vt[tb0:tb0 + L, :].rearrange("(j p) n -> p j n", p=128), reads=["vt"], writes=["va"])
            for grp in range(8):
                I0 = grp * 4
                tsl = slice(I0 * 128, (I0 + 4) * 128)
                oi = gi % 2
                gi += 1
                S.dma("sp", osb[oi][:], ot[tb0 + I0 * 128:tb0 + (I0 + 4) * 128, :].rearrange("(j p) n -> p j n", p=128), reads=["ot"], writes=["osb%d" % oi])
                for d in range(2):
                    Js = list(range(0, I0 + 4)) if d == 0 else list(range(I0, 32))
                    for J in Js:
                        js = slice(J * 128, (J + 1) * 128)
                        p = pi % 2
                        pi += 1
                        ab, sb_ = p, 2 + p
                        S.op("pe", lambda e, d=d, js=js, ab=ab: e.matmul(ps[ab][:, :], lhsT=Lt[d][0:33, js], rhs=R[d][0:33, tsl], start=True, stop=True),
                             reads=["L%d" % d, "R%d" % d], writes=[psk[ab]])
                        S.op("pe", lambda e, js=js, sb_=sb_: e.matmul(ps[sb_][:, :], lhsT=kT[:, js], rhs=qT[:, tsl], start=True, stop=True),
                             reads=["kT", "qT"], writes=[psk[sb_]])
                        k = J - I0
                        if 0 <= k <= 3:
                            S.op("dve", lambda e, p=p, ab=ab, d=d, k=k: e.tensor_tensor(out=Dt[p][:], in0=ps[ab][:, :], in1=mp[:, d * 4 + k, :], op=ALU.add),
                                 reads=[psk[ab], "mp"], writes=["Dt%d" % p])
                            S.op("act", lambda e, p=p: e.activation(out=Dt[p][:], in_=Dt[p][:], func=AF.Exp), reads=["Dt%d" % p], writes=["Dt%d" % p])
                        else:
                            S.op("act", lambda e, p=p, ab=ab: e.activation(out=Dt[p][:], in_=ps[ab][:, :], func=AF.Exp), reads=[psk[ab]], writes=["Dt%d" % p])
                        S.op("dve", lambda e, p=p, sb_=sb_: e.tensor_tensor(out=PT[p][:], in0=ps[sb_][:, :], in1=Dt[p][:], op=ALU.mult),
                             reads=[psk[sb_], "Dt%d" % p], writes=["PT%d" % p])
                        for i in range(4):
                            I = I0 + i
                            valid = (J <= I) if d == 0 else (J >= I)
                            if not valid:
                                continue
                            is_first = (J == 0) if d == 0 else (J == I)
                            is_last = (J == I) if d == 0 else (J == 31)
                            S.op("pe", lambda e, p=p, J=J, i=i, is_first=is_first, is_last=is_last: e.matmul(ps[4 + i][:, 0:258], lhsT=PT[p][:, i * 128:(i + 1) * 128], rhs=va[:, J, :], start=is_first, stop=is_last),
                                 reads=["PT%d" % p, "va"], writes=[psk[4 + i]], inc=True)
                    for i in range(4):
                        accb = 4 + i
                        S.op("dve", lambda e, accb=accb: e.tensor_scalar(out=den2[:, 0:1], in0=ps[accb][:, 256:257], scalar1=-1.0, scalar2=1.0, op0=ALU.mult, op1=ALU.max), reads=[psk[accb]], writes=["den2"])
                        S.op("dve", lambda e, accb=accb: e.tensor_scalar(out=den2[:, 1:2], in0=ps[accb][:, 256:257], scalar1=1.0, scalar2=None, op0=ALU.max), reads=[psk[accb]], writes=["den2"])
                        S.op("dve", lambda e, i=i: e.tensor_tensor(out=den[:, i:i + 1], in0=den2[:, 0:1], in1=den2[:, 1:2], op=ALU.max), reads=["den2"], writes=["den"])
                        S.op("dve", lambda e, i=i: e.reciprocal(out=den[:, i:i + 1], in_=den[:, i:i + 1]), reads=["den"], writes=["den"])
                        if d == 0:
                            S.op("dve", lambda e, accb=accb, i=i: e.tensor_scalar(out=hacc[:, i, :], in0=ps[accb][:, 0:256], scalar1=den[:, i:i + 1], scalar2=None, op0=ALU.mult), reads=[psk[accb], "den"], writes=["hacc"])
                        else:
                            S.op("dve", lambda e, accb=accb, i=i: e.scalar_tensor_tensor(out=hacc[:, i, :], in0=ps[accb][:, 0:256], scalar=den[:, i:i + 1], in1=hacc[:, i, :], op0=ALU.mult, op1=ALU.add), reads=[psk[accb], "den", "hacc"], writes=["hacc"])
                S.op("act", lambda e, oi=oi: e.activation(out=osb[oi][:], in_=osb[oi][:], func=AF.Sigmoid), reads=["osb%d" % oi], writes=["osb%d" % oi])
                for i in range(4):
                    if dbg is not None:
                        S.dma("sp", dbg[1][tb0 + (I0 + i) * 128:tb0 + (I0 + i + 1) * 128, :], hacc[:, i, :], reads=["hacc"], writes=["dbg_h"])
                    S.op("dve", lambda e, i=i: e.bn_stats(out=st6[:], in_=hacc[:, i, :]), reads=["hacc"], writes=["st6"])
                    S.op("dve", lambda e: e.bn_aggr(out=mv[:, 0:2], in_=st6[:]), reads=["st6"], writes=["mv"])
                    S.op("act", lambda e: e.activation(out=mv[:, 2:3], in_=mv[:, 1:2], func=AF.Sqrt, bias=1e-5), reads=["mv"], writes=["mv"])
                    S.op("dve", lambda e: e.reciprocal(out=mv[:, 3:4], in_=mv[:, 2:3]), reads=["mv"], writes=["mv"])
                    S.op("dve", lambda e, i=i: e.tensor_scalar(out=hacc[:, i, :], in0=hacc[:, i, :], scalar1=mv[:, 0:1], scalar2=mv[:, 3:4], op0=ALU.subtract, op1=ALU.mult), reads=["hacc", "mv"], writes=["hacc"])
                    S.op("pool", lambda e, i=i: e.tensor_tensor(out=hacc[:, i, :], in0=hacc[:, i, :], in1=mlgs[:], op=ALU.mult), reads=["hacc", "mlgs"], writes=["hacc"])
                S.op("pool", lambda e, oi=oi: e.tensor_tensor(out=yo[oi][:], in0=hacc[:], in1=osb[oi][:], op=ALU.mult), reads=["hacc", "osb%d" % oi], writes=["yo%d" % oi])
                S.dma("act", yml[tb0 + I0 * 128:tb0 + (I0 + 4) * 128, :].rearrange("(j p) n -> p j n", p=128), yo[oi][:], reads=["yo%d" % oi], writes=["yml"])


COL_Q = 6144
COL_K = COL_Q + 1024
COL_V = COL_K + 1024
COL_O = COL_V + 2048
COL_IF = COL_O + 2048
COL_GATE = COL_IF + 32


def p1_in_maps(inp):
    f32 = np.float32
    x = np.asarray(inp["x"], dtype=f32).reshape(NTOK, D)
    xT = np.ascontiguousarray(x.T)
    w_in = np.asarray(inp["w_in"], dtype=f32)
    dft = _dft_consts()
    featsT = _feats_T()
    deltas = _deltas()
    tl = np.linspace(0.0, 1.0, L, dtype=f32)
    tnorm = np.ascontiguousarray(tl.reshape(32, 128).T)
    identf = np.eye(128, dtype=f32)
    identb = identf.astype(ml_dtypes.bfloat16)
    ii = np.arange(128)
    maskf = np.where(ii[:, None] <= ii[None, :], 0.0, -30000.0).astype(f32)
    maskb = np.where(ii[:, None] >= ii[None, :], 0.0, -30000.0).astype(f32)
    mpat = np.zeros((128, 8, 512), f32)
    for k in range(4):
        for i in range(4):
            blkf = maskf if i == k else (np.full((128, 128), -30000.0, f32) if i < k else np.zeros((128, 128), f32))
            blkb = maskb if i == k else (np.full((128, 128), -30000.0, f32) if i > k else np.zeros((128, 128), f32))
            mpat[:, k, i * 128:(i + 1) * 128] = blkf
            mpat[:, 4 + k, i * 128:(i + 1) * 128] = blkb
    conv_w = np.asarray(inp["hy_conv_w"], dtype=f32)
    conv_b = np.asarray(inp["hy_conv_b"], dtype=f32)
    hy_bias = np.asarray(inp["hy_bias"], dtype=f32)
    wout_full = np.asarray(inp["hy_f_wout"], dtype=f32).reshape(64, 2, 2, 2048)
    gate_bias = np.asarray(inp["ml_gate_bias"], dtype=f32)
    ml_g = np.asarray(inp["ml_norm_g"], dtype=f32)
    fvec = np.stack([np.asarray(inp[k], dtype=f32) for k in ("hy_f_b1", "hy_f_fr1", "hy_f_b2", "hy_f_fr2", "hy_f_b3", "hy_f_fr3")], axis=1)
    maps = []
    for c in range(NCORE):
        ch = np.arange(c * 256, (c + 1) * 256)
        cols = np.concatenate([ch, 2048 + ch, 4096 + ch,
                               COL_Q + c * 128 + np.arange(128), COL_K + c * 128 + np.arange(128),
                               COL_V + c * 256 + np.arange(256), COL_O + c * 256 + np.arange(256),
                               COL_IF + np.arange(4) * 8 + c])
        wc = np.ascontiguousarray(w_in[:, cols])
        hc = np.concatenate([ch, 2048 + ch, 4096 + ch])
        cwa = np.concatenate([conv_w[:, hc], conv_b[None, hc]], axis=0)
        cw = np.ascontiguousarray(cwa.reshape(4, 6, 128).transpose(2, 1, 0))
        hyb = np.ascontiguousarray(hy_bias[:, ch].reshape(2, 2, 128).transpose(2, 1, 0))
        wo = np.ascontiguousarray(wout_full[:, :, :, ch].transpose(0, 2, 1, 3).reshape(64, 1024))
        gb = np.zeros((8, 2), f32)
        for d in range(2):
            for b in range(NB):
                gb[d * 4 + b, 0] = gate_bias[2 * d, c]
                gb[d * 4 + b, 1] = gate_bias[2 * d + 1, c]
        maps.append(dict(
            xT=xT, wc=wc, cw=cw, hyb=hyb, featsT=featsT,
            fw1=np.asarray(inp["hy_f_w1"], dtype=f32), fw2=np.asarray(inp["hy_f_w2"], dtype=f32),
            fw3=np.asarray(inp["hy_f_w3"], dtype=f32), fvec=np.ascontiguousarray(fvec), wout=wo,
            delB=np.ascontiguousarray(np.broadcast_to(deltas[ch][None, :], (128, 256))), tnorm=tnorm,
            cfwd=dft["cfwd"], sfwd=dft["sfwd"], cinv=dft["cinv"], sinv=dft["sinv"],
            identb=identb, identf=identf, gbias=gb,
            mlg=np.ascontiguousarray(np.broadcast_to(ml_g[c * 256:(c + 1) * 256][None, :], (128, 256))),
            mpat=mpat))
    return maps


TPC = NTOK // NCORE
ALPHA = 2.0 ** 0.25
LN_EPS = 1e-5


def _cast_rows(nc, S, es_parent, jobs, width):
    with ExitStack() as es:
        st = [es.enter_context(nc.sbuf_tensor("cst%d" % i, [128, width], F32)) for i in range(2)]
        cb = [es.enter_context(nc.sbuf_tensor("ccb%d" % i, [128, width], BF16)) for i in range(2)]
        engs = ["act", "dve", "pool"]
        for n, (src, dstfn, w) in enumerate(jobs):
            i = n % 2
            S.dma("sp", st[i][:, 0:w], src, writes=["cst%d" % i])
            e = engs[n % 3]
            if e == "act":
                S.op("act", lambda en, i=i, w=w: en.copy(out=cb[i][:, 0:w], in_=st[i][:, 0:w]), reads=["cst%d" % i], writes=["ccb%d" % i])
            else:
                S.op(e, lambda en, i=i, w=w: en.tensor_copy(out=cb[i][:, 0:w], in_=st[i][:, 0:w]), reads=["cst%d" % i], writes=["ccb%d" % i])
            dst, view = dstfn(cb[i])
            S.dma("act", dst, view, reads=["ccb%d" % i], writes=["castout%d" % (n % 8)])
    S.barrier()


def build_p2a():
    nc = bass.Bass("TRN2", target_bir_lowering=False)
    S = Sched(nc)

    def din(name, shape, dt=F32):
        return nc.dram_tensor(name, list(shape), dt, kind="ExternalInput").ap()

    yhT = din("yhT", [2048, TPC])
    ymT = din("ymT", [2048, TPC])
    xTs = din("xTs", [D, TPC])
    xs = din("xs", [TPC, D])
    wg = din("wg", [D, 2 * D])
    phy = din("phy", [2048, D])
    pml = din("pml", [2048, D])
    wo = din("wo", [D, D])
    ln1g = din("ln1g", [128, D])
    ln1b = din("ln1b", [128, D])
    rw = din("rw", [D, 72])
    rb = din("rb", [128, 72])
    iota8 = din("iota8", [128, 8])
    identf_d = din("identf", [128, 128])

    h1o = nc.dram_tensor("h1o", [TPC, D], F32, kind="ExternalOutput").ap()
    rinfo = nc.dram_tensor("rinfo", [TPC, 8], F32, kind="ExternalOutput").ap()

    yh_s = nc.dram_tensor("yh_s", [4, 128, 16, 512], BF16).ap()
    ym_s = nc.dram_tensor("ym_s", [4, 128, 16, 512], BF16).ap()
    xT_s = nc.dram_tensor("xT_s", [4, 128, 32, 512], BF16).ap()
    ph_s = nc.dram_tensor("ph_s", [32, 128, 16, 128], BF16).ap()
    pm_s = nc.dram_tensor("pm_s", [32, 128, 16, 128], BF16).ap()
    wg_s = nc.dram_tensor("wg_s", [64, 128, 32, 128], BF16).ap()
    wo_s = nc.dram_tensor("wo_s", [8, 128, 32, 512], BF16).ap()
    mT_s = nc.dram_tensor("mT_s", [16, 128, 32, 128], BF16).ap()
    mix_s = nc.dram_tensor("mix_s", [TPC, D], F32).ap()

    ps = [nc.alloc_psum_tensor("ps%d" % i, [128, 512], F32) for i in range(8)]
    psk = ["ps%d" % i for i in range(8)]
    identf = nc.alloc_sbuf_tensor("identf_s", [128, 128], F32)
    S.dma("sp", identf[:], identf_d, writes=["identf"])

    jobs = []
    for kc in range(16):
        jobs.append((yhT[kc * 128:(kc + 1) * 128, :], (lambda t, kc=kc: (yh_s[:, :, kc, :].rearrange("tb p c -> p tb c"), t[:, 0:2048].rearrange("p (tb c) -> p tb c", tb=4))), 2048))
        jobs.append((ymT[kc * 128:(kc + 1) * 128, :], (lambda t, kc=kc: (ym_s[:, :, kc, :].rearrange("tb p c -> p tb c"), t[:, 0:2048].rearrange("p (tb c) -> p tb c", tb=4))), 2048))
    for kc in range(32):
        jobs.append((xTs[kc * 128:(kc + 1) * 128, :], (lambda t, kc=kc: (xT_s[:, :, kc, :].rearrange("tb p c -> p tb c"), t[:, 0:2048].rearrange("p (tb c) -> p tb c", tb=4))), 2048))
    for kc in range(16):
        jobs.append((phy[kc * 128:(kc + 1) * 128, :], (lambda t, kc=kc: (ph_s[:, :, kc, :].rearrange("ct p c -> p ct c"), t[:, 0:4096].rearrange("p (ct c) -> p ct c", ct=32))), 4096))
        jobs.append((pml[kc * 128:(kc + 1) * 128, :], (lambda t, kc=kc: (pm_s[:, :, kc, :].rearrange("ct p c -> p ct c"), t[:, 0:4096].rearrange("p (ct c) -> p ct c", ct=32))), 4096))
    for kc in range(32):
        for hf in range(2):
            jobs.append((wg[kc * 128:(kc + 1) * 128, hf * 4096:(hf + 1) * 4096],
                         (lambda t, kc=kc, hf=hf: (wg_s[hf * 32:(hf + 1) * 32, :, kc, :].rearrange("ct p c -> p ct c"), t[:, 0:4096].rearrange("p (ct c) -> p ct c", ct=32))), 4096))
        jobs.append((wo[kc * 128:(kc + 1) * 128, :], (lambda t, kc=kc: (wo_s[:, :, kc, :].rearrange("dt p c -> p dt c"), t[:, 0:4096].rearrange("p (dt c) -> p dt c", dt=8))), 4096))
    _cast_rows(nc, S, None, jobs, 4096)

    with ExitStack() as es:
        yh = es.enter_context(nc.sbuf_tensor("yh", [128, 16, 512], BF16))
        ym = es.enter_context(nc.sbuf_tensor("ym", [128, 16, 512], BF16))
        xb = es.enter_context(nc.sbuf_tensor("xb", [128, 32, 512], BF16))
        wph = [es.enter_context(nc.sbuf_tensor("wph%d" % i, [128, 16, 128], BF16)) for i in range(2)]
        wpm = [es.enter_context(nc.sbuf_tensor("wpm%d" % i, [128, 16, 128], BF16)) for i in range(2)]
        wgh = [es.enter_context(nc.sbuf_tensor("wgh%d" % i, [128, 32, 128], BF16)) for i in range(2)]
        wgm = [es.enter_context(nc.sbuf_tensor("wgm%d" % i, [128, 32, 128], BF16)) for i in range(2)]
        sgh = es.enter_context(nc.sbuf_tensor("sgh", [128, 512], F32))
        sgm = es.enter_context(nc.sbuf_tensor("sgm", [128, 512], F32))
        ta = es.enter_context(nc.sbuf_tensor("ta", [128, 512], F32))
        tbv = es.enter_context(nc.sbuf_tensor("tbv", [128, 512], F32))
        mo = [es.enter_context(nc.sbuf_tensor("mo%d" % i, [128, 512], BF16)) for i in range(2)]

        def loadw(ct):
            i = ct % 2
            S.dma("sp", wph[i][:], ph_s[ct], writes=["wph%d" % i])
            S.dma("sp", wpm[i][:], pm_s[ct], writes=["wpm%d" % i])
            S.dma("sp", wgh[i][:], wg_s[ct], writes=["wgh%d" % i])
            S.dma("sp", wgm[i][:], wg_s[32 + ct], writes=["wgm%d" % i])

        for tb in range(4):
            S.dma("sp", yh[:], yh_s[tb], writes=["yh"])
            S.dma("sp", ym[:], ym_s[tb], writes=["ym"])
            S.dma("sp", xb[:], xT_s[tb], writes=["xb"])
            loadw(0)
            for ct in range(32):
                i = ct % 2
                if ct + 1 < 32:
                    loadw(ct + 1)
                b0 = (ct % 2) * 4
                for kc in range(16):
                    S.op("pe", lambda e, kc=kc, i=i, b0=b0: e.matmul(ps[b0][:, :], lhsT=wph[i][:, kc, :], rhs=yh[:, kc, :], start=(kc == 0), stop=(kc == 15)),
                         reads=["wph%d" % i, "yh"], writes=[psk[b0]], inc=(kc == 15))
                for kc in range(16):
                    S.op("pe", lambda e, kc=kc, i=i, b0=b0: e.matmul(ps[b0 + 1][:, :], lhsT=wpm[i][:, kc, :], rhs=ym[:, kc, :], start=(kc == 0), stop=(kc == 15)),
                         reads=["wpm%d" % i, "ym"], writes=[psk[b0 + 1]], inc=(kc == 15))
                for kc in range(32):
                    S.op("pe", lambda e, kc=kc, i=i, b0=b0: e.matmul(ps[b0 + 2][:, :], lhsT=wgh[i][:, kc, :], rhs=xb[:, kc, :], start=(kc == 0), stop=(kc == 31)),
                         reads=["wgh%d" % i, "xb"], writes=[psk[b0 + 2]], inc=(kc == 31))
                for kc in range(32):
                    S.op("pe", lambda e, kc=kc, i=i, b0=b0: e.matmul(ps[b0 + 3][:, :], lhsT=wgm[i][:, kc, :], rhs=xb[:, kc, :], start=(kc == 0), stop=(kc == 31)),
                         reads=["wgm%d" % i, "xb"], writes=[psk[b0 + 3]], inc=(kc == 31))
                S.op("act", lambda e, b0=b0: e.activation(out=sgh[:], in_=ps[b0 + 2][:, :], func=AF.Sigmoid), reads=[psk[b0 + 2]], writes=["sgh"])
                S.op("act", lambda e, b0=b0: e.activation(out=sgm[:], in_=ps[b0 + 3][:, :], func=AF.Sigmoid), reads=[psk[b0 + 3]], writes=["sgm"])
                S.op("dve", lambda e, b0=b0: e.tensor_tensor(out=ta[:], in0=ps[b0][:, :], in1=sgh[:], op=ALU.mult), reads=[psk[b0], "sgh"], writes=["ta"])
                S.op("dve", lambda e, b0=b0: e.tensor_tensor(out=tbv[:], in0=ps[b0 + 1][:, :], in1=sgm[:], op=ALU.mult), reads=[psk[b0 + 1], "sgm"], writes=["tbv"])
                S.op("pool", lambda e, i=i: e.tensor_tensor(out=mo[i][:], in0=ta[:], in1=tbv[:], op=ALU.add), reads=["ta", "tbv"], writes=["mo%d" % i])
                S.dma("act", mT_s[tb * 4:(tb + 1) * 4, :, ct, :].rearrange("tt p c -> p tt c"), mo[i][:].rearrange("p (tt c) -> p tt c", tt=4), reads=["mo%d" % i], writes=["mT_s"])
    S.barrier()

    with ExitStack() as es:
        wop = [es.enter_context(nc.sbuf_tensor("wop%d" % i, [128, 32, 512], BF16)) for i in range(2)]
        mt = [es.enter_context(nc.sbuf_tensor("mt%d" % i, [128, 32, 128], BF16)) for i in range(2)]
        mxo = [es.enter_context(nc.sbuf_tensor("mxo%d" % i, [128, 512], F32)) for i in range(2)]
        S.dma("sp", wop[0][:], wo_s[0], writes=["wop0"])
        n = 0
        for dt in range(8):
            wi = dt % 2
            if dt + 1 < 8:
                S.dma("sp", wop[(dt + 1) % 2][:], wo_s[dt + 1], writes=["wop%d" % ((dt + 1) % 2)])
            for tt in range(16):
                i = n % 2
                n += 1
                S.dma("sp", mt[i][:], mT_s[tt], reads=["mT_s"], writes=["mt%d" % i])
                bank = i
                for kc in range(32):
                    S.op("pe", lambda e, kc=kc, i=i, wi=wi, bank=bank: e.matmul(ps[bank][:, :], lhsT=mt[i][:, kc, :], rhs=wop[wi][:, kc, :], start=(kc == 0), stop=(kc == 31)),
                         reads=["mt%d" % i, "wop%d" % wi], writes=[psk[bank]], inc=(kc == 31))
                if i == 0:
                    S.op("act", lambda e, i=i, bank=bank: e.copy(out=mxo[i][:], in_=ps[bank][:, :]), reads=[psk[bank]], writes=["mxo%d" % i])
                else:
                    S.op("dve", lambda e, i=i, bank=bank: e.tensor_copy(out=mxo[i][:], in_=ps[bank][:, :]), reads=[psk[bank]], writes=["mxo%d" % i])
                S.dma("act", mix_s[tt * 128:(tt + 1) * 128, dt * 512:(dt + 1) * 512], mxo[i][:], reads=["mxo%d" % i], writes=["mix_s"])
    S.barrier()

    with ExitStack() as es:
        gB = es.enter_context(nc.sbuf_tensor("gB", [128, D], F32))
        bB = es.enter_context(nc.sbuf_tensor("bB", [128, D], F32))
        rws = es.enter_context(nc.sbuf_tensor("rws", [128, 32, 72], F32))
        rbs = es.enter_context(nc.sbuf_tensor("rbs", [128, 72], F32))
        io8 = es.enter_context(nc.sbuf_tensor("io8", [128, 8], F32))
        rt = [es.enter_context(nc.sbuf_tensor("rt%d" % i, [128, D], F32)) for i in range(2)]
        xt = [es.enter_context(nc.sbuf_tensor("xt%d" % i, [128, D], F32)) for i in range(2)]
        hT = es.enter_context(nc.sbuf_tensor("hT", [128, 32, 128], F32))
        st = es.enter_context(nc.sbuf_tensor("st", [128, 8, 6], F32))
        mv = es.enter_context(nc.sbuf_tensor("mv", [128, 4], F32))
        S.dma("sp", gB[:], ln1g, writes=["gB"])
        S.dma("sp", bB[:], ln1b, writes=["bB"])
        S.dma("sp", rws[:], rw.rearrange("(kc p) n -> p kc n", p=128), writes=["rws"])
        S.dma("sp", rbs[:], rb, writes=["rbs"])
        S.dma("sp", io8[:], iota8, writes=["io8"])
        R = _RouterTiles(nc, es)
        for tt in range(16):
            i = tt % 2
            S.dma("sp", rt[i][:], mix_s[tt * 128:(tt + 1) * 128, :], reads=["mix_s"], writes=["rt%d" % i])
            S.dma("sp", xt[i][:], xs[tt * 128:(tt + 1) * 128, :], writes=["xt%d" % i])
            _ln_tile(S, rt[i], "rt%d" % i, xt[i], "xt%d" % i, ALPHA, st, mv, gB, bB)
            S.dma("act", h1o[tt * 128:(tt + 1) * 128, :], rt[i][:], reads=["rt%d" % i], writes=["h1o"])
            for q in range(8):
                bank = 4 + (q % 2)
                for j in range(4):
                    kc = q * 4 + j
                    S.op("pe", lambda e, i=i, kc=kc, j=j, bank=bank: e.transpose(out=ps[bank][:, j * 128:(j + 1) * 128], in_=rt[i][:, kc * 128:(kc + 1) * 128], identity=identf[:]),
                         reads=["rt%d" % i, "identf"], writes=[psk[bank]], inc=(j == 3))
                if q % 2 == 0:
                    S.op("act", lambda e, q=q, bank=bank: e.copy(out=hT[:, q * 4:(q + 1) * 4, :], in_=ps[bank][:, :].rearrange("p (j n) -> p j n", j=4)), reads=[psk[bank]], writes=["hT"])
                else:
                    S.op("dve", lambda e, q=q, bank=bank: e.tensor_copy(out=hT[:, q * 4:(q + 1) * 4, :], in_=ps[bank][:, :].rearrange("p (j n) -> p j n", j=4)), reads=[psk[bank]], writes=["hT"])
            for kc in range(32):
                S.op("pe", lambda e, kc=kc: e.matmul(ps[6][:, 0:72], lhsT=hT[:, kc, :], rhs=rws[:, kc, :], start=(kc == 0), stop=(kc == 31)),
                     reads=["hT", "rws"], writes=[psk[6]], inc=(kc == 31))
            _router_tile(S, R, ps[6], psk[6], rbs, io8)
            S.dma("act", rinfo[tt * 128:(tt + 1) * 128, :], R.info[:], reads=["r_info"], writes=["rinfo"])
    S.barrier()
    S.finish(["h1o", "rinfo"])
    return nc


def _ln_tile(S, rt, rk, xt, xk, alpha, st, mv, gB, bB):
    if xt is not None:
        S.op("dve", lambda e: e.scalar_tensor_tensor(out=rt[:], in0=xt[:], scalar=alpha, in1=rt[:], op0=ALU.mult, op1=ALU.add), reads=[xk, rk], writes=[rk])
    for c in range(8):
        S.op("dve", lambda e, c=c: e.bn_stats(out=st[:, c, :], in_=rt[:, c * 512:(c + 1) * 512]), reads=[rk], writes=["st"])
    S.op("dve", lambda e: e.bn_aggr(out=mv[:, 0:2], in_=st[:].rearrange("p c s -> p (c s)")), reads=["st"], writes=["mv"])
    S.op("act", lambda e: e.activation(out=mv[:, 2:3], in_=mv[:, 1:2], func=AF.Sqrt, bias=LN_EPS), reads=["mv"], writes=["mv"])
    S.op("dve", lambda e: e.reciprocal(out=mv[:, 3:4], in_=mv[:, 2:3]), reads=["mv"], writes=["mv"])
    S.op("dve", lambda e: e.tensor_scalar(out=rt[:], in0=rt[:], scalar1=mv[:, 0:1], scalar2=mv[:, 3:4], op0=ALU.subtract, op1=ALU.mult), reads=[rk, "mv"], writes=[rk])
    S.op("pool", lambda e: e.tensor_tensor(out=rt[:], in0=rt[:], in1=gB[:], op=ALU.mult), reads=[rk, "gB"], writes=[rk])
    S.op("pool", lambda e: e.tensor_tensor(out=rt[:], in0=rt[:], in1=bB[:], op=ALU.add), reads=[rk, "bB"], writes=[rk])


class _RouterTiles:
    def __init__(self, nc, es):
        def t(name, shape):
            return es.enter_context(nc.sbuf_tensor(name, shape, F32))
        self.lg = t("r_lg", [128, 72])
        self.m = t("r_m", [128, 8])
        self.oh = t("r_oh", [128, 8])
        self.e1 = t("r_e1", [128, 8])
        self.t64 = t("r_t64", [128, 64])
        self.sel = t("r_sel", [128, 8])
        self.sel2 = t("r_sel2", [128, 8])
        self.oa = t("r_oa", [128, 8])
        self.ob = t("r_ob", [128, 8])
        self.tmp8 = t("r_tmp8", [128, 8])
        self.info = t("r_info", [128, 8])


def _router_tile(S, R, psl, pk, rbs, io8):
    K = "r_w"
    def dve(fn, reads=(), writes=()):
        S.op("dve", fn, reads=[K] + list(reads), writes=[K] + list(writes))
    def act(fn, reads=(), writes=()):
        S.op("act", fn, reads=[K] + list(reads), writes=[K] + list(writes))
    lg, m, oh, e1, t64, sel, sel2, oa, ob, tmp8, info = R.lg, R.m, R.oh, R.e1, R.t64, R.sel, R.sel2, R.oa, R.ob, R.tmp8, R.info
    dve(lambda e: e.tensor_tensor(out=lg[:], in0=psl[:, 0:72], in1=rbs[:], op=ALU.add), reads=[pk, "rbs"])
    dve(lambda e: e.tensor_reduce(out=m[:, 0:1], in_=lg[:, 0:8], axis=AX.X, op=ALU.max))
    dve(lambda e: e.tensor_scalar(out=oh[:], in0=lg[:, 0:8], scalar1=m[:, 0:1], scalar2=None, op0=ALU.is_equal))
    dve(lambda e: e.tensor_scalar(out=e1[:], in0=lg[:, 0:8], scalar1=m[:, 0:1], scalar2=None, op0=ALU.subtract))
    act(lambda e: e.activation(out=e1[:], in_=e1[:], func=AF.Exp))
    dve(lambda e: e.tensor_reduce(out=m[:, 1:2], in_=e1[:], axis=AX.X, op=ALU.add))
    dve(lambda e: e.reciprocal(out=m[:, 2:3], in_=m[:, 1:2]))
    dve(lambda e: e.tensor_tensor(out=tmp8[:], in0=oh[:], in1=io8[:], op=ALU.mult), reads=["io8"])
    dve(lambda e: e.tensor_reduce(out=info[:, 0:1], in_=tmp8[:], axis=AX.X, op=ALU.add), writes=["r_info"])
    dve(lambda e: e.tensor_scalar(out=sel[:], in0=lg[:, 8:16], scalar1=oh[:, 0:1], scalar2=None, op0=ALU.mult))
    for g in range(1, 8):
        dve(lambda e, g=g: e.scalar_tensor_tensor(out=sel[:], in0=lg[:, 8 + g * 8:16 + g * 8], scalar=oh[:, g:g + 1], in1=sel[:], op0=ALU.mult, op1=ALU.add))
    dve(lambda e: e.tensor_reduce(out=m[:, 3:4], in_=sel[:], axis=AX.X, op=ALU.max))
    dve(lambda e: e.tensor_scalar(out=oa[:], in0=sel[:], scalar1=m[:, 3:4], scalar2=None, op0=ALU.is_equal))
    dve(lambda e: e.scalar_tensor_tensor(out=sel2[:], in0=oa[:], scalar=-1e30, in1=sel[:], op0=ALU.mult, op1=ALU.add))
    dve(lambda e: e.tensor_reduce(out=m[:, 4:5], in_=sel2[:], axis=AX.X, op=ALU.max))
    dve(lambda e: e.tensor_scalar(out=ob[:], in0=sel2[:], scalar1=m[:, 4:5], scalar2=None, op0=ALU.is_equal))
    dve(lambda e: e.tensor_tensor(out=tmp8[:], in0=oa[:], in1=io8[:], op=ALU.mult), reads=["io8"])
    dve(lambda e: e.tensor_reduce(out=info[:, 1:2], in_=tmp8[:], axis=AX.X, op=ALU.add), writes=["r_info"])
    dve(lambda e: e.tensor_tensor(out=tmp8[:], in0=ob[:], in1=io8[:], op=ALU.mult), reads=["io8"])
    dve(lambda e: e.tensor_reduce(out=info[:, 2:3], in_=tmp8[:], axis=AX.X, op=ALU.add), writes=["r_info"])
    dve(lambda e: e.tensor_tensor(out=m[:, 6:7], in0=m[:, 4:5], in1=m[:, 3:4], op=ALU.subtract))
    act(lambda e: e.activation(out=m[:, 6:7], in_=m[:, 6:7], func=AF.Exp))
    dve(lambda e: e.tensor_scalar(out=m[:, 6:7], in0=m[:, 6:7], scalar1=1.0, scalar2=None, op0=ALU.add))
    dve(lambda e: e.reciprocal(out=m[:, 5:6], in_=m[:, 6:7]))
    dve(lambda e: e.tensor_scalar(out=m[:, 7:8], in0=m[:, 5:6], scalar1=-1.0, scalar2=1.0, op0=ALU.mult, op1=ALU.add))
    dve(lambda e: e.tensor_tensor(out=info[:, 3:4], in0=m[:, 5:6], in1=m[:, 2:3], op=ALU.mult), writes=["r_info"])
    dve(lambda e: e.tensor_tensor(out=info[:, 4:5], in0=m[:, 7:8], in1=m[:, 2:3], op=ALU.mult), writes=["r_info"])
    dve(lambda e: e.memset(info[:, 5:8], 0.0), writes=["r_info"])


CAP = 2560
SBK = 256
NSB = CAP // SBK
HID = 768


def build_p2b():
    nc = bass.Bass("TRN2", target_bir_lowering=False)
    S = Sched(nc)

    def din(name, shape, dt=F32):
        return nc.dram_tensor(name, list(shape), dt, kind="ExternalInput").ap()

    hgT = din("hgT", [D, CAP])
    hg = din("hg", [CAP, D])
    wrow = din("wrow", [8, CAP])
    w1g = din("w1g", [8, D, HID])
    w3g = din("w3g", [8, D, HID])
    w2g = din("w2g", [8, HID, D])
    ln2g = din("ln2g", [128, D])
    ln2b = din("ln2b", [128, D])
    og = nc.dram_tensor("og", [CAP, D], F32, kind="ExternalOutput").ap()

    w1_s = nc.dram_tensor("w1_s", [8, 6, 128, 32, 128], BF16).ap()
    w3_s = nc.dram_tensor("w3_s", [8, 6, 128, 32, 128], BF16).ap()
    w2_s = nc.dram_tensor("w2_s", [8, 2, 128, 24, 512], BF16).ap()
    hT_s = nc.dram_tensor("hT_s", [NSB, 128, 32, SBK], BF16).ap()

    ps = [nc.alloc_psum_tensor("ps%d" % i, [128, 512], F32) for i in range(8)]
    psk = ["ps%d" % i for i in range(8)]

    jobs = []
    for kc in range(32):
        jobs.append((hgT[kc * 128:(kc + 1) * 128, :], (lambda t, kc=kc: (hT_s[:, :, kc, :].rearrange("sb p c -> p sb c"), t[:, 0:CAP].rearrange("p (sb c) -> p sb c", sb=NSB))), CAP))
    for e in range(8):
        for kc in range(32):
            jobs.append((w1g[e, kc * 128:(kc + 1) * 128, :], (lambda t, e=e, kc=kc: (w1_s[e, :, :, kc, :].rearrange("ft p c -> p ft c"), t[:, 0:HID].rearrange("p (ft c) -> p ft c", ft=6))), HID))
            jobs.append((w3g[e, kc * 128:(kc + 1) * 128, :], (lambda t, e=e, kc=kc: (w3_s[e, :, :, kc, :].rearrange("ft p c -> p ft c"), t[:, 0:HID].rearrange("p (ft c) -> p ft c", ft=6))), HID))
        for ft in range(6):
            jobs.append((w2g[e, ft * 128:(ft + 1) * 128, :], (lambda t, e=e, ft=ft: (w2_s[:, e // 4, :, (e % 4) * 6 + ft, :].rearrange("dt p c -> p dt c"), t[:, 0:D].rearrange("p (dt c) -> p dt c", dt=8))), D))
    _cast_rows(nc, S, None, jobs, 4096)

    with ExitStack() as es:
        gB = es.enter_context(nc.sbuf_tensor("gB", [128, D], F32))
        bB = es.enter_context(nc.sbuf_tensor("bB", [128, D], F32))
        hTb = es.enter_context(nc.sbuf_tensor("hTb", [128, 32, SBK], BF16))
        wrb = es.enter_context(nc.sbuf_tensor("wrb", [128, 8, SBK], F32))
        HTw = es.enter_context(nc.sbuf_tensor("HTw", [128, 48, SBK], BF16))
        w1p = [es.enter_context(nc.sbuf_tensor("w1p%d" % i, [128, 32, 128], BF16)) for i in range(2)]
        w3p = [es.enter_context(nc.sbuf_tensor("w3p%d" % i, [128, 32, 128], BF16)) for i in range(2)]
        w2p = [es.enter_context(nc.sbuf_tensor("w2p%d" % i, [128, 24, 512], BF16)) for i in range(2)]
        rt = [es.enter_context(nc.sbuf_tensor("rt%d" % i, [128, D], F32)) for i in range(2)]
        sa = es.enter_context(nc.sbuf_tensor("sa", [128, SBK], F32))
        sb2 = es.enter_context(nc.sbuf_tensor("sb2", [128, SBK], F32))
        st = es.enter_context(nc.sbuf_tensor("st", [128, 8, 6], F32))
        mv = es.enter_context(nc.sbuf_tensor("mv", [128, 4], F32))
        S.dma("sp", gB[:], ln2g, writes=["gB"])
        S.dma("sp", bB[:], ln2b, writes=["bB"])
        nw = 0
        for sb in range(NSB):
            s0 = sb * SBK
            S.dma("sp", hTb[:], hT_s[sb], writes=["hTb"])
            S.dma("sp", wrb[:], wrow[:, s0:s0 + SBK].partition_broadcast(128), writes=["wrb"])
            for st_ in range(2):
                S.dma("sp", rt[st_][:], hg[s0 + st_ * 128:s0 + (st_ + 1) * 128, :], writes=["rt%d" % st_])
            for e in range(8):
                for ft in range(6):
                    i = nw % 2
                    nw += 1
                    S.dma("sp", w1p[i][:], w1_s[e, ft], writes=["w1p%d" % i])
                    S.dma("sp", w3p[i][:], w3_s[e, ft], writes=["w3p%d" % i])
                    ba, bb = 2 * i, 2 * i + 1
                    for kc in range(32):
                        S.op("pe", lambda en, kc=kc, i=i, ba=ba: en.matmul(ps[ba][:, 0:SBK], lhsT=w1p[i][:, kc, :], rhs=hTb[:, kc, :], start=(kc == 0), stop=(kc == 31)),
                             reads=["w1p%d" % i, "hTb"], writes=[psk[ba]], inc=(kc == 31))
                    for kc in range(32):
                        S.op("pe", lambda en, kc=kc, i=i, bb=bb: en.matmul(ps[bb][:, 0:SBK], lhsT=w3p[i][:, kc, :], rhs=hTb[:, kc, :], start=(kc == 0), stop=(kc == 31)),
                             reads=["w3p%d" % i, "hTb"], writes=[psk[bb]], inc=(kc == 31))
                    S.op("act", lambda en, ba=ba: en.activation(out=sa[:], in_=ps[ba][:, 0:SBK], func=AF.Silu), reads=[psk[ba]], writes=["sa"])
                    S.op("dve", lambda en, bb=bb: en.tensor_tensor(out=sb2[:], in0=ps[bb][:, 0:SBK], in1=sa[:], op=ALU.mult), reads=[psk[bb], "sa"], writes=["sb2"])
                    S.op("pool", lambda en, e=e, ft=ft: en.tensor_tensor(out=HTw[:, e * 6 + ft, :], in0=sb2[:], in1=wrb[:, e, :], op=ALU.mult), reads=["sb2", "wrb"], writes=["HTw"])
            for dt in range(8):
                for hf in range(2):
                    S.dma("sp", w2p[hf][:], w2_s[dt, hf], writes=["w2p%d" % hf])
                for st_ in range(2):
                    bank = 4 + st_
                    for hf in range(2):
                        for j in range(24):
                            S.op("pe", lambda en, hf=hf, j=j, st_=st_, bank=bank: en.matmul(ps[bank][:, :], lhsT=HTw[:, hf * 24 + j, st_ * 128:(st_ + 1) * 128], rhs=w2p[hf][:, j, :],
                                                                                      start=(hf == 0 and j == 0), stop=(hf == 1 and j == 23)),
                                 reads=["HTw", "w2p%d" % hf], writes=[psk[bank]], inc=(hf == 1 and j == 23))
                    S.op("dve", lambda en, st_=st_, bank=bank, dt=dt: en.scalar_tensor_tensor(out=rt[st_][:, dt * 512:(dt + 1) * 512], in0=rt[st_][:, dt * 512:(dt + 1) * 512], scalar=ALPHA, in1=ps[bank][:, :], op0=ALU.mult, op1=ALU.add),
                         reads=["rt%d" % st_, psk[bank]], writes=["rt%d" % st_])
            for st_ in range(2):
                _ln_tile(S, rt[st_], "rt%d" % st_, None, None, None, st, mv, gB, bB)
                S.dma("act", og[s0 + st_ * 128:s0 + (st_ + 1) * 128, :], rt[st_][:], reads=["rt%d" % st_], writes=["og"])
    S.barrier()
    S.finish(["og"])
    return nc


def p2a_in_maps(inp, yhyT, yml):
    f32 = np.float32
    x = np.asarray(inp["x"], dtype=f32).reshape(NTOK, D)
    w_in = np.asarray(inp["w_in"], dtype=f32)
    wg = np.ascontiguousarray(w_in[:, COL_GATE:])
    phy = np.asarray(inp["p_hy"], dtype=f32)
    pml = np.asarray(inp["p_ml"], dtype=f32)
    wo = np.asarray(inp["w_out"], dtype=f32)
    ln1g = np.ascontiguousarray(np.broadcast_to(np.asarray(inp["ln1_g"], dtype=f32)[None, :], (128, D)))
    ln1b = np.ascontiguousarray(np.broadcast_to(np.asarray(inp["ln1_b"], dtype=f32)[None, :], (128, D)))
    rw = np.ascontiguousarray(np.concatenate([np.asarray(inp["router_w1"], dtype=f32), np.asarray(inp["router_w2"], dtype=f32)], axis=1))
    rbv = np.concatenate([np.asarray(inp["router_b1"], dtype=f32), np.asarray(inp["router_b2"], dtype=f32)])
    rb = np.ascontiguousarray(np.broadcast_to(rbv[None, :], (128, 72)))
    iota8 = np.ascontiguousarray(np.broadcast_to(np.arange(8, dtype=f32)[None, :], (128, 8)))
    identf = np.eye(128, dtype=f32)
    maps = []
    for c in range(NCORE):
        sl = slice(c * TPC, (c + 1) * TPC)
        xs = np.ascontiguousarray(x[sl])
        maps.append(dict(
            yhT=np.ascontiguousarray(yhyT[:, sl]), ymT=np.ascontiguousarray(yml[sl].T),
            xTs=np.ascontiguousarray(xs.T), xs=xs, wg=wg, phy=phy, pml=pml, wo=wo,
            ln1g=ln1g, ln1b=ln1b, rw=rw, rb=rb, iota8=iota8, identf=identf))
    return maps


def p2b_in_maps(inp, h1, rinfo):
    f32 = np.float32
    grp = np.rint(rinfo[:, 0]).astype(np.int64)
    ja = np.rint(rinfo[:, 1]).astype(np.int64)
    jb = np.rint(rinfo[:, 2]).astype(np.int64)
    ln2g = np.ascontiguousarray(np.broadcast_to(np.asarray(inp["ln2_g"], dtype=f32)[None, :], (128, D)))
    ln2b = np.ascontiguousarray(np.broadcast_to(np.asarray(inp["ln2_b"], dtype=f32)[None, :], (128, D)))
    maps, idxs = [], []
    for g in range(NCORE):
        idx = np.nonzero(grp == g)[0]
        n = len(idx)
        if n > CAP:
            raise RuntimeError("group %d has %d tokens > capacity %d" % (g, n, CAP))
        hgm = np.zeros((CAP, D), f32)
        hgm[:n] = h1[idx]
        wrow = np.zeros((8, CAP), f32)
        ar = np.arange(n)
        wrow[ja[idx], ar] = rinfo[idx, 3]
        wrow[jb[idx], ar] = rinfo[idx, 4]
        maps.append(dict(hgT=np.ascontiguousarray(hgm.T), hg=hgm, wrow=wrow,
                         w1g=np.asarray(inp["exp_w1"][g * 8:(g + 1) * 8], dtype=f32),
                         w3g=np.asarray(inp["exp_w3"][g * 8:(g + 1) * 8], dtype=f32),
                         w2g=np.asarray(inp["exp_w2"][g * 8:(g + 1) * 8], dtype=f32),
                         ln2g=ln2g, ln2b=ln2b))
        idxs.append(idx)
    return maps, idxs


def run_phase1(inp):
    maps = p1_in_maps(inp)
    nc = build_p1()
    res = run_bass_kernel_spmd(nc, maps, core_ids=list(range(NCORE)))
    yhyT = np.concatenate([r["yhyT"] for r in res.results], axis=0)
    yml = np.concatenate([r["yml"] for r in res.results], axis=1)
    return yhyT, yml


def run_phase2a(inp, yhyT, yml):
    maps = p2a_in_maps(inp, yhyT, yml)
    nc = build_p2a()
    res = run_bass_kernel_spmd(nc, maps, core_ids=list(range(NCORE)))
    h1 = np.concatenate([r["h1o"] for r in res.results], axis=0)
    rinfo = np.concatenate([r["rinfo"] for r in res.results], axis=0)
    return h1, rinfo


def run_phase2b(inp, h1, rinfo):
    maps, idxs = p2b_in_maps(inp, h1, rinfo)
    nc = build_p2b()
    res = run_bass_kernel_spmd(nc, maps, core_ids=list(range(NCORE)))
    out = np.zeros((NTOK, D), np.float32)
    for g in range(NCORE):
        idx = idxs[g]
        out[idx] = res.results[g]["og"][:len(idx)]
    return out


def kernel(**inputs):
    yhyT, yml = run_phase1(inputs)
    h1, rinfo = run_phase2a(inputs, yhyT, yml)
    out = run_phase2b(inputs, h1, rinfo)
    return out.reshape(NB, L, D).astype(np.float32)
```
